# Optimizing a Trainium2 kernel written in Bass

```python
import math
import jax, jax.numpy as jnp
from jax import lax
import numpy as np

D_MODEL = 2048
BATCH = 2
SEQ = 4096
DEPTH = 2

GRID_W = 64
ROPE_THETA = 10000.0
HEAD_DIM = 128
N_Q_HEADS = 8
N_KV_HEADS = 2
ATTN_W = N_Q_HEADS * HEAD_DIM
KV_W = N_KV_HEADS * HEAD_DIM
Q_BLOCK = 128
SSM_GROUP = 16
SSM_GROUPS = 64
SSM_W = SSM_GROUP * SSM_GROUPS
SSM_STATE = 64
DT_MIN = 1e-3
DT_MAX = 1e-1
MLP_CHUNK = 128
MLP_GROUPS = 8
MLP_GROUP_W = 128
MLP_W = MLP_GROUPS * MLP_GROUP_W
N_BRANCH = 3
BRANCH_W = 1024
N_IN = ATTN_W + 2 * KV_W + SSM_W + 2 * MLP_W + N_BRANCH * D_MODEL
D_FF = 5632
N_EXPERTS = 8
TOP_K = 2
D_FF_EXPERT = 7168
MOE_BLOCK = 256
N_DENSE = (DEPTH + 1) // 2
N_MOE = DEPTH // 2
PLE_DIM = 256
EPS = 1e-6

kernel_name = 'hybrid_gqa_s5_gmlp_moe_encoder'


def rms_norm(x, g):
    xf = x.astype(jnp.float32)
    y = xf * lax.rsqrt(jnp.mean(xf * xf, axis=-1, keepdims=True) + EPS)
    return (y * g.astype(jnp.float32)).astype(x.dtype)


def swiglu(h, w1, w3, w2):
    return (jax.nn.silu(h @ w1) * (h @ w3)) @ w2


def axial_rope_tables(L):
    rows = L // GRID_W
    t = jnp.arange(L)
    pos = jnp.stack([t // GRID_W - rows // 2, t % GRID_W - GRID_W // 2], axis=-1).astype(jnp.float32)
    n_freq = HEAD_DIM // 4
    inv_freq = ROPE_THETA ** (-jnp.arange(n_freq, dtype=jnp.float32) / n_freq)
    ang = pos[:, :, None] * inv_freq
    return jnp.cos(ang), jnp.sin(ang)


def apply_axial_rope(x, cos, sin):
    B, L, H, _ = x.shape
    xr = x.astype(jnp.float32).reshape(B, L, H, 2, 2, HEAD_DIM // 4)
    x1, x2 = xr[..., 0, :], xr[..., 1, :]
    c = cos[None, :, None]
    s = sin[None, :, None]
    out = jnp.stack([x1 * c - x2 * s, x2 * c + x1 * s], axis=-2)
    return out.reshape(B, L, H, HEAD_DIM).astype(x.dtype)


def axial_gqa_attention(q, k, v, q_gain, k_gain):
    B, L, _ = q.shape
    q = rms_norm(q.reshape(B, L, N_Q_HEADS, HEAD_DIM), q_gain)
    k = rms_norm(k.reshape(B, L, N_KV_HEADS, HEAD_DIM), k_gain)
    v = v.reshape(B, L, N_KV_HEADS, HEAD_DIM)
    cos, sin = axial_rope_tables(L)
    q = apply_axial_rope(q, cos, sin)
    k = apply_axial_rope(k, cos, sin)
    rep = N_Q_HEADS // N_KV_HEADS
    nb = L // Q_BLOCK
    qb = q.reshape(B, nb, Q_BLOCK, N_KV_HEADS, rep, HEAD_DIM).transpose(1, 0, 2, 3, 4, 5)
    scale = HEAD_DIM ** -0.5

    def block(qblk):
        s = jnp.einsum('bqgrd,bkgd->bgrqk', qblk, k, preferred_element_type=jnp.float32) * scale
        pr = jax.nn.softmax(s, axis=-1)
        return jnp.einsum('bgrqk,bkgd->bqgrd', pr.astype(v.dtype), v)

    o = lax.map(block, qb)
    return o.transpose(1, 0, 2, 3, 4, 5).reshape(B, L, ATTN_W)


def _ssm_combine(left, right):
    a_l, b_l = left
    a_r, b_r = right
    return a_r * a_l, a_r * b_l + b_r


def bidirectional_s5(u, lam_re, lam_im, log_dt, b_re, b_im, c_re, c_im, d_skip, w_glu):
    B, L, _ = u.shape
    uf = u.astype(jnp.float32).reshape(B, L, SSM_GROUPS, SSM_GROUP)
    lam = lax.complex(lam_re.astype(jnp.float32), lam_im.astype(jnp.float32))
    dt = jnp.exp(log_dt.astype(jnp.float32))[..., None]
    lam_bar = jnp.exp(lam * dt)
    b_cplx = lax.complex(b_re.astype(jnp.float32), b_im.astype(jnp.float32))
    b_bar = ((lam_bar - 1.0) / lam)[..., None] * b_cplx
    y = jnp.zeros((B, L, SSM_GROUPS, SSM_GROUP), jnp.float32)
    for dn, rev in ((0, False), (1, True)):
        bu = lax.complex(jnp.einsum('blgc,gpc->blgp', uf, jnp.real(b_bar[dn])),
                         jnp.einsum('blgc,gpc->blgp', uf, jnp.imag(b_bar[dn])))
        a = jnp.broadcast_to(lam_bar[dn], bu.shape)
        _, states = lax.associative_scan(_ssm_combine, (a, bu), axis=1, reverse=rev)
        y = y + jnp.einsum('blgp,gcp->blgc', jnp.real(states), c_re[dn].astype(jnp.float32)) \
              - jnp.einsum('blgp,gcp->blgc', jnp.imag(states), c_im[dn].astype(jnp.float32))
    y = y.reshape(B, L, SSM_W) + d_skip.astype(jnp.float32) * uf.reshape(B, L, SSM_W)
    y = jax.nn.gelu(y)
    y = y * jax.nn.sigmoid(y @ w_glu.astype(jnp.float32))
    return y.astype(u.dtype)


def chunked_spatial_gating(z_u, z_v, v_gain, w_s, b_s):
    B, L, _ = z_u.shape
    u = jax.nn.gelu(z_u)
    v = rms_norm(jax.nn.gelu(z_v), v_gain)
    nc = L // MLP_CHUNK
    vc = v.reshape(B, nc, MLP_CHUNK, MLP_GROUPS, MLP_GROUP_W)
    s = jnp.einsum('bnpgc,gqp->bnqgc', vc, w_s) + b_s.T[None, None, :, :, None]
    return u * s.reshape(B, L, MLP_W)


def mixer_sublayer(h, w_in, q_gain, k_gain, lam_re, lam_im, log_dt, b_re, b_im, c_re, c_im,
                   d_skip, w_glu, v_gain, w_s, b_s, w_branch, w_out):
    B, L, D = h.shape
    proj = h @ w_in
    cuts = [int(c) for c in np.cumsum([ATTN_W, KV_W, KV_W, SSM_W, MLP_W, MLP_W])]
    q, k, v, u_ssm, z_u, z_v, gate_logits = jnp.split(proj, cuts, axis=-1)
    attn = axial_gqa_attention(q, k, v, q_gain, k_gain)
    ssm = bidirectional_s5(u_ssm, lam_re, lam_im, log_dt, b_re, b_im, c_re, c_im, d_skip, w_glu)
    mlp = chunked_spatial_gating(z_u, z_v, v_gain, w_s, b_s)
    gates = jax.nn.sigmoid(gate_logits.astype(jnp.float32)).reshape(B, L, N_BRANCH, D)
    branches = jnp.stack([attn, ssm, mlp], axis=2)
    proj_b = jnp.einsum('blnc,ncd->blnd', branches, w_branch)
    merged = jnp.sum(gates * proj_b.astype(jnp.float32), axis=2).astype(h.dtype)
    return merged @ w_out


def moe_swiglu(h, w_router, e_w1, e_w3, e_w2):
    B, L, D = h.shape
    t = h.reshape(-1, D)
    N = t.shape[0]
    logits = (t @ w_router).astype(jnp.float32)
    top_val, top_idx = lax.top_k(logits, TOP_K)
    gates = jax.nn.softmax(top_val, axis=-1)
    NK = N * TOP_K
    e_flat = top_idx.reshape(-1)
    tok_flat = jnp.arange(NK) // TOP_K
    g_flat = gates.reshape(-1)
    order = jnp.argsort(e_flat)
    e_s, tok_s, g_s = e_flat[order], tok_flat[order], g_flat[order]
    counts = jnp.bincount(e_flat, length=N_EXPERTS)
    starts = jnp.cumsum(counts) - counts
    padded = (counts + MOE_BLOCK - 1) // MOE_BLOCK * MOE_BLOCK
    pends = jnp.cumsum(padded)
    pstarts = pends - padded
    dest = pstarts[e_s] + (jnp.arange(NK) - starts[e_s])
    n_blocks = -(-NK // MOE_BLOCK) + N_EXPERTS
    cap = n_blocks * MOE_BLOCK
    xbuf = jnp.zeros((cap, D), t.dtype).at[dest].set(t[tok_s])
    block_e = jnp.minimum(jnp.searchsorted(pends, jnp.arange(n_blocks) * MOE_BLOCK, side='right'), N_EXPERTS - 1)

    def expert_block(args):
        xb, e = args
        return swiglu(xb, e_w1[e], e_w3[e], e_w2[e])

    ybuf = lax.map(expert_block, (xbuf.reshape(n_blocks, MOE_BLOCK, D), block_e)).reshape(cap, D)
    y = ybuf[dest] * g_s[:, None].astype(ybuf.dtype)
    out = jnp.zeros((N, D), t.dtype).at[tok_s].add(y.astype(t.dtype))
    return out.reshape(B, L, D)


def setup_inputs(seed: int = 0) -> dict:
    key = jax.random.key(seed)
    ks = iter(jax.random.split(key, 40))
    f32 = jnp.float32

    def nrm(shape, scale):
        return jax.random.normal(next(ks), shape, f32) * scale

    def gain(shape):
        return 1.0 + nrm(shape, 0.01)

    ssm_shape = (DEPTH, 2, SSM_GROUPS, SSM_STATE)
    return {
        'x': nrm((BATCH, SEQ, D_MODEL), 1.0),
        'p': nrm((DEPTH, BATCH, SEQ, PLE_DIM), 1.0),
        'mix_norm': gain((DEPTH, D_MODEL)),
        'w_in': nrm((DEPTH, D_MODEL, N_IN), D_MODEL ** -0.5),
        'q_norm': gain((DEPTH, HEAD_DIM)),
        'k_norm': gain((DEPTH, HEAD_DIM)),
        'ssm_lambda_re': -0.5 + nrm(ssm_shape, 0.01),
        'ssm_lambda_im': jnp.pi * jnp.arange(SSM_STATE, dtype=f32) + nrm(ssm_shape, 0.01),
        'ssm_log_dt': jax.random.uniform(next(ks), (DEPTH, 2, SSM_GROUPS), f32, math.log(DT_MIN), math.log(DT_MAX)),
        'ssm_b_re': nrm((DEPTH, 2, SSM_GROUPS, SSM_STATE, SSM_GROUP), (2 * SSM_GROUP) ** -0.5),
        'ssm_b_im': nrm((DEPTH, 2, SSM_GROUPS, SSM_STATE, SSM_GROUP), (2 * SSM_GROUP) ** -0.5),
        'ssm_c_re': nrm((DEPTH, 2, SSM_GROUPS, SSM_GROUP, SSM_STATE), (2 * SSM_STATE) ** -0.5),
        'ssm_c_im': nrm((DEPTH, 2, SSM_GROUPS, SSM_GROUP, SSM_STATE), (2 * SSM_STATE) ** -0.5),
        'ssm_d': nrm((DEPTH, SSM_W), 1.0),
        'ssm_glu_w': nrm((DEPTH, SSM_W, SSM_W), SSM_W ** -0.5),
        'gmlp_v_norm': gain((DEPTH, MLP_W)),
        'gmlp_ws': nrm((DEPTH, MLP_GROUPS, MLP_CHUNK, MLP_CHUNK), MLP_CHUNK ** -0.5),
        'gmlp_b': 1.0 + nrm((DEPTH, MLP_GROUPS, MLP_CHUNK), 0.1),
        'w_branch': nrm((DEPTH, N_BRANCH, BRANCH_W, D_MODEL), BRANCH_W ** -0.5),
        'w_out': nrm((DEPTH, D_MODEL, D_MODEL), D_MODEL ** -0.5),
        'ffn_norm': gain((DEPTH, D_MODEL)),
        'dense_w1': nrm((N_DENSE, D_MODEL, D_FF), D_MODEL ** -0.5),
        'dense_w3': nrm((N_DENSE, D_MODEL, D_FF), D_MODEL ** -0.5),
        'dense_w2': nrm((N_DENSE, D_FF, D_MODEL), D_FF ** -0.5),
        'router_w': nrm((N_MOE, D_MODEL, N_EXPERTS), D_MODEL ** -0.5),
        'expert_w1': nrm((N_MOE, N_EXPERTS, D_MODEL, D_FF_EXPERT), D_MODEL ** -0.5),
        'expert_w3': nrm((N_MOE, N_EXPERTS, D_MODEL, D_FF_EXPERT), D_MODEL ** -0.5),
        'expert_w2': nrm((N_MOE, N_EXPERTS, D_FF_EXPERT, D_MODEL), D_FF_EXPERT ** -0.5),
        'ple_norm': gain((DEPTH, D_MODEL)),
        'ple_gate_w': nrm((DEPTH, D_MODEL, D_MODEL), D_MODEL ** -0.5),
        'ple_proj_w': nrm((DEPTH, PLE_DIM, D_MODEL), PLE_DIM ** -0.5),
    }


def reference(x, p, mix_norm, w_in, q_norm, k_norm, ssm_lambda_re, ssm_lambda_im, ssm_log_dt,
              ssm_b_re, ssm_b_im, ssm_c_re, ssm_c_im, ssm_d, ssm_glu_w, gmlp_v_norm, gmlp_ws, gmlp_b,
              w_branch, w_out, ffn_norm, dense_w1, dense_w3, dense_w2, router_w, expert_w1, expert_w3,
              expert_w2, ple_norm, ple_gate_w, ple_proj_w):
    for i in range(DEPTH):
        h = rms_norm(x, mix_norm[i])
        x = x + mixer_sublayer(h, w_in[i], q_norm[i], k_norm[i], ssm_lambda_re[i], ssm_lambda_im[i],
                               ssm_log_dt[i], ssm_b_re[i], ssm_b_im[i], ssm_c_re[i], ssm_c_im[i], ssm_d[i],
                               ssm_glu_w[i], gmlp_v_norm[i], gmlp_ws[i], gmlp_b[i], w_branch[i], w_out[i])
        h = rms_norm(x, ffn_norm[i])
        j = i // 2
        if i % 2 == 0:
            x = x + swiglu(h, dense_w1[j], dense_w3[j], dense_w2[j])
        else:
            x = x + moe_swiglu(h, router_w[j], expert_w1[j], expert_w3[j], expert_w2[j])
        g = jax.nn.sigmoid((rms_norm(x, ple_norm[i]) @ ple_gate_w[i]).astype(jnp.float32))
        x = x + (g * (p[i] @ ple_proj_w[i]).astype(jnp.float32)).astype(x.dtype)
    return x
```

```python
import math
from contextlib import ExitStack

import numpy as np
import ml_dtypes
import concourse.bass as bass
import concourse.mybir as mybir
from concourse.bass_utils import run_bass_kernel_spmd

F32 = mybir.dt.float32
BF16 = mybir.dt.bfloat16
ALU = mybir.AluOpType
AF = mybir.ActivationFunctionType
AX = mybir.AxisListType

D = 2048
KC = 16
NTOK = 1024
SEQ = 4096
N_IN = 10752
EPS = 1e-6
NCORES = 8


class Tok:
    __slots__ = ("lw", "rd", "name", "sem", "cnt")

    def __init__(self, name=""):
        self.lw = None
        self.rd = []
        self.name = name
        self.sem = None
        self.cnt = 0


class Prog:
    ENG = ("pe", "dve", "act", "pool", "sp")

    def __init__(self, nc, stack):
        self.nc = nc
        self.stack = stack
        self.q = {e: [] for e in self.ENG}
        self.sem = {e: stack.enter_context(nc.semaphore("s_" + e)) for e in self.ENG}
        self.cnt = {e: 0 for e in self.ENG}
        self.waited = {e: {} for e in self.ENG}
        self.nsem = 0
        self.final = []
        self.pool = []
        self.free = []
        self.scopes = []

    def _deps(self, eng, reads, writes):
        deps = []
        for b in reads:
            if b.lw is not None:
                deps.append(b.lw)
        for b in writes:
            if b.lw is not None:
                deps.append(b.lw)
            deps.extend(b.rd)
        w = self.waited[eng]
        best = {}
        own = self.sem[eng]
        for (sem, val) in deps:
            if eng == "pe" and sem is own:
                continue
            k = id(sem)
            if w.get(k, 0) < val and best.get(k, (None, 0))[1] < val:
                best[k] = (sem, val)
        waits = []
        for k, (sem, val) in best.items():
            w[k] = val
            waits.append((sem, val))
        return waits

    def _mark(self, tok, reads, writes):
        for b in writes:
            b.lw = tok
            b.rd = []
        for b in reads:
            if b not in writes:
                b.rd.append(tok)
                if len(b.rd) > 48:
                    best = {}
                    for (sem, val) in b.rd:
                        k = id(sem)
                        if best.get(k, (None, 0))[1] < val:
                            best[k] = (sem, val)
                    b.rd = list(best.values())

    def op(self, eng, fn, reads=(), writes=()):
        waits = self._deps(eng, reads, writes)
        self.cnt[eng] += 1
        tok = (self.sem[eng], self.cnt[eng])
        self.q[eng].append((waits, fn, (self.sem[eng], 1)))
        self._mark(tok, reads, writes)
        return tok

    def dma(self, eng, out, in_, owner, reads=(), writes=(), final=False, **kw):
        waits = self._deps(eng, reads, writes)
        if owner.sem is None:
            if self.free:
                idx = self.free.pop()
            else:
                self.nsem += 1
                self.pool.append([self.stack.enter_context(self.nc.semaphore("d%d" % self.nsem)), 0])
                idx = len(self.pool) - 1
            owner.sem = self.pool[idx][0]
            owner.cnt = self.pool[idx][1]
            owner.name = idx
            if self.scopes:
                self.scopes[-1].append(owner)
        owner.cnt += 16
        self.pool[owner.name][1] = owner.cnt
        tok = (owner.sem, owner.cnt)
        self.q[eng].append((waits, (lambda e: e.dma_start(out=out, in_=in_, **kw)), (owner.sem, 16)))
        self._mark(tok, reads, writes)
        if final:
            self.final.append(tok)
        return tok

    def barrier(self):
        toks = [(self.sem[e], self.cnt[e]) for e in self.ENG if self.cnt[e] > 0]
        toks += [(p[0], p[1]) for p in self.pool if p[1] > 0]
        for e in self.ENG:
            waits = []
            w = self.waited[e]
            for (sem, val) in toks:
                if sem is self.sem[e]:
                    continue
                if w.get(id(sem), 0) < val:
                    w[id(sem)] = val
                    waits.append((sem, val))
            if waits:
                self.q[e].append((waits, None, None))

    def push_scope(self):
        self.scopes.append([])

    def pop_scope(self):
        self.barrier()
        for t in self.scopes.pop():
            self.free.append(t.name)
            t.sem = None

    def finish(self):
        best = {}
        for (sem, val) in self.final:
            k = id(sem)
            if best.get(k, (None, 0))[1] < val:
                best[k] = (sem, val)
        self.q["sp"].append((list(best.values()), None, None))

    def emit(self):
        nc = self.nc
        q = self.q

        def replay(name, e):
            for (waits, fn, inc) in q[name]:
                for (sem, val) in waits:
                    e.wait_ge(sem, val)
                if fn is not None:
                    ins = fn(e)
                    if inc is not None:
                        ins.then_inc(inc[0], inc[1])

        with nc.Block() as block:
            @block.tensor
            def _(e):
                replay("pe", e)

            @block.vector
            def _(e):
                replay("dve", e)

            @block.scalar
            def _(e):
                replay("act", e)

            @block.gpsimd
            def _(e):
                replay("pool", e)

            @block.sync
            def _(e):
                replay("sp", e)


def MM(out, lhsT, rhs, start=True, stop=True):
    return lambda e: e.matmul(out, lhsT, rhs, start=start, stop=stop)


def ACTF(out, in_, func, **kw):
    return lambda e: e.activation(out, in_, func, **kw)


def TT(out, a, b, op):
    return lambda e: e.tensor_tensor(out, a, b, op)


def TS(out, a, s1, s2, op0, op1=None):
    if op1 is None:
        return lambda e: e.tensor_scalar(out, a, s1, None, op0)
    return lambda e: e.tensor_scalar(out, a, s1, s2, op0, op1)


def STT(out, in0, scalar, in1, op0, op1):
    return lambda e: e.scalar_tensor_tensor(out, in0, scalar, in1, op0, op1)


def CP(out, in_):
    return lambda e: e.tensor_copy(out, in_)


def MSET(out, v):
    return lambda e: e.memset(out, v)


def RCP(out, in_):
    return lambda e: e.reciprocal(out, in_)


TB = 512
NBLK = SEQ // TB
WS_KC = 16
WS_W = 512


class KB:
    def __init__(self):
        self.nc = bass.Bass("TRN2", target_bir_lowering=False)
        self.st = ExitStack()
        self.P = Prog(self.nc, self.st)
        self.n = 0
        self.bank = [self.st.enter_context(self.nc.psum_tensor("bank%d" % i, [128, 512], F32)) for i in range(8)]
        self.btok = [Tok("bank%d" % i) for i in range(8)]
        self.sstack = [self.st]

    def push(self):
        st = ExitStack()
        self.sstack.append(st)
        self.P.push_scope()

    def pop(self):
        self.P.pop_scope()
        self.sstack.pop().close()

    def sb(self, shape, dt, name=None):
        self.n += 1
        return self.sstack[-1].enter_context(
            self.nc.sbuf_tensor("sb%d_%s" % (self.n, name or "t"), shape, dt))

    def din(self, name, shape, dt=F32):
        return self.nc.dram_tensor(name, list(shape), dt, kind="ExternalInput").ap()

    def dout(self, name, shape, dt=F32):
        return self.nc.dram_tensor(name, list(shape), dt, kind="ExternalOutput").ap()

    def dscr(self, name, shape, dt=F32):
        return self.nc.dram_tensor(name, list(shape), dt).ap()

    def load(self, dram_ap, shape, dt=F32, eng="sp", name=None):
        t = self.sb(shape, dt, name)
        tk = Tok(name or "ld")
        self.P.dma(eng, t[:], dram_ap, tk, writes=[tk])
        return t, tk

    def done(self):
        self.P.finish()
        self.P.emit()
        self.st.close()
        return self.nc

    def init_wslots(self, n=4, width=WS_W):
        self.ws_w = width
        self.wslots = [self.sb([128, WS_KC, width], BF16, "wslot%d" % i) for i in range(n)]
        self.wtok = [Tok("wslot%d" % i) for i in range(n)]
        self.wi = 0

    def load_w(self, w_ap, kc_n, c0, cw):
        i = self.wi % len(self.wslots)
        self.wi += 1
        t, tk = self.wslots[i], self.wtok[i]
        wv = w_ap.rearrange("(kc p) n -> p kc n", p=128)
        for q in range(0, kc_n, 8):
            q1 = min(q + 8, kc_n)
            self.P.dma("pool", t[:, q:q1, 0:cw], wv[:, q:q1, c0:c0 + cw], tk, writes=[tk])
        return t, tk

    def linear(self, w_ap, kc_n, col0, ncols, rhs_fn, rhs_toks, epilogue, banks):
        P = self.P
        bi = 0
        for g0 in range(col0, col0 + ncols, self.ws_w):
            gw = min(self.ws_w, col0 + ncols - g0)
            wt, wtk = self.load_w(w_ap, kc_n, g0, gw)
            for jj in range(gw // 128):
                b = banks[bi % len(banks)]
                bi += 1
                for kc in range(kc_n):
                    P.op("pe", MM(self.bank[b][:, :], wt[:, kc, jj * 128:(jj + 1) * 128], rhs_fn(kc),
                                  start=(kc == 0), stop=(kc == kc_n - 1)),
                         reads=[wtk] + list(rhs_toks), writes=[self.btok[b]])
                epilogue((g0 + jj * 128) // 128, b)

    def linear2(self, w_ap, kc_n, col0, ncols, rhs_fn, rhs_toks, epilogue, banks, nsb):
        P = self.P
        bi = 0
        for g0 in range(col0, col0 + ncols, self.ws_w):
            gw = min(self.ws_w, col0 + ncols - g0)
            wt, wtk = self.load_w(w_ap, kc_n, g0, gw)
            for jj in range(gw // 128):
                for sb in range(nsb):
                    b = banks[bi % len(banks)]
                    bi += 1
                    for kc in range(kc_n):
                        P.op("pe", MM(self.bank[b][:, :], wt[:, kc, jj * 128:(jj + 1) * 128], rhs_fn(kc, sb),
                                      start=(kc == 0), stop=(kc == kc_n - 1)),
                             reads=[wtk, rhs_toks[sb]], writes=[self.btok[b]])
                    epilogue((g0 + jj * 128) // 128, b, sb)

    def consts(self):
        P = self.P
        self.ones32 = self.sb([128, 128], F32, "ones32")
        self.ones_tok = Tok("ones")
        P.op("dve", MSET(self.ones32[:, :], 1.0), writes=[self.ones_tok])
        self.onesb = self.sb([128, 128], BF16, "onesb")
        self.onesb_tok = Tok("onesb")
        P.op("dve", MSET(self.onesb[:, :], 1.0), writes=[self.onesb_tok])
        self.eps_t = self.sb([128, 1], F32, "eps")
        self.eps_tok = Tok("eps")
        P.op("dve", MSET(self.eps_t[:, :], EPS), writes=[self.eps_tok])
        self.sq = [self.sb([128, TB], F32, "sq%d" % i) for i in range(2)]
        self.sqtok = [Tok("sq%d" % i) for i in range(2)]
        self.rstd = self.sb([128, TB], F32, "rstd")
        self.rstok = Tok("rstd")

    def rmsnorm(self, xT, xtok, gcols, gtok, hT, htok, bank, kcn=KC, h32cb=None):
        P = self.P
        sq, sqtok, rstd, rstok = self.sq, self.sqtok, self.rstd, self.rstok
        for kc in range(kcn):
            s = kc % 2
            P.op("act", ACTF(sq[s][:, :], xT[:, kc, :], AF.Square), reads=[xtok], writes=[sqtok[s]])
            P.op("pe", MM(self.bank[bank][:, :], self.ones32[:, :], sq[s][:, :], start=(kc == 0), stop=(kc == kcn - 1)),
                 reads=[sqtok[s], self.ones_tok], writes=[self.btok[bank]])
        P.op("act", ACTF(rstd[:, :], self.bank[bank][:, :], AF.Sqrt, scale=1.0 / (kcn * 128), bias=self.eps_t[:, 0:1]),
             reads=[self.btok[bank], self.eps_tok], writes=[rstok])
        P.op("dve", RCP(rstd[:, :], rstd[:, :]), reads=[rstok], writes=[rstok])
        for kc in range(kcn):
            P.op("dve", STT(hT[:, kc, :], xT[:, kc, :], gcols[:, kc:kc + 1], rstd[:, :], ALU.mult, ALU.mult),
                 reads=[xtok, gtok, rstok], writes=[htok])
            if h32cb is not None:
                h32cb(kc)


def rev_view(ap2d, n):
    pstep = ap2d.ap[0][0]
    return bass.AP(ap2d.tensor, ap2d.offset + (n - 1), [[pstep, 128], [-1, n]])


def build_program(depth=2, dbg=False):
    kb = KB()
    nc, P = kb.nc, kb.P
    L = 2
    xT_d = kb.din("xT", [D, SEQ])
    pT_d = kb.din("pT", [L, 256, SEQ])
    w_in_d = kb.din("w_in", [L, D, N_IN])
    gmix_d = kb.din("gmix", [L, 128, KC])
    gffn_d = kb.din("gffn", [L, 128, KC])
    gple_d = kb.din("gple", [L, 128, KC])
    qg_d = kb.din("qg", [L, 128, 2])
    rc_d = kb.din("ropeC", [128, SEQ])
    rs_d = kb.din("ropeS", [128, SEQ])
    pm_d = kb.din("perm", [128, 128])
    id_d = kb.din("ident", [128, 128])
    iota_d = kb.din("iota", [128, 2, 512])
    lre_d = kb.din("lam_re_c", [L, 128, 64])
    lim_d = kb.din("lam_im_c", [L, 128, 64])
    ldt_d = kb.din("logdt_c", [L, 128, 64])
    bt_d = kb.din("BT", [L, 64, 2, 128, 128])
    ct_d = kb.din("CT", [L, 64, 2, 128, 128])
    dsk_d = kb.din("dskip", [L, 128, 8])
    glu_d = kb.din("glu_w", [L, 1024, 1024])
    vg_d = kb.din("vgain", [L, 128, 1024])
    wst_d = kb.din("wsT", [L, 128, 8, 128])
    bs_d = kb.din("bsrow", [L, 1, 1024])
    wbr_d = kb.din("w_branch", [L, 3072, D])
    wout_d = kb.din("w_out", [L, D, D])
    dw1_d = kb.din("dense_w1", [D, 5632])
    dw3_d = kb.din("dense_w3", [D, 5632])
    dw2_d = kb.din("dense_w2", [5632, D])
    rw_d = kb.din("router_w", [128, KC, 128])
    if depth >= 2:
        ew1_d = kb.din("expert_w1", [8, D, 7168])
        ew3_d = kb.din("expert_w3", [8, D, 7168])
        ew2_d = kb.din("expert_w2", [8, 7168, D])
    else:
        ew1_d = ew3_d = ew2_d = None
    pg_d = kb.din("ple_gate_w", [L, D, D])
    pp_d = kb.din("ple_proj_w", [L, 256, D])
    out_d = kb.dout("outT", [D, SEQ])
    kb.dbg = None
    if dbg:
        kb.dbg = (kb.dout("dbg_x1", [D, TB]), kb.dout("dbg_x2", [D, TB]), kb.dout("dbg_g", [128, 8 * TB]), kb.dout("dbg_lg", [128, 32]))
    qk_s = kb.dscr("qk_s", [1280, SEQ], BF16)
    V_s = kb.dscr("V_s", [SEQ, 256], BF16)
    u_s = kb.dscr("u_s", [1024, SEQ], F32)
    zu_s = kb.dscr("zu_s", [1024, SEQ], F32)
    vn_s = kb.dscr("vn_s", [SEQ, 1024], BF16)
    g_s = kb.dscr("g_s", [6144, SEQ], F32)
    at_s = kb.dscr("at_s", [1024, SEQ], BF16)
    y_s = kb.dscr("y_s", [1024, SEQ], F32)
    x_s = kb.dscr("x_s", [D, SEQ], F32)
    x1_s = kb.dscr("x1_s", [D, SEQ], F32)

    kb.consts()
    pm, pmtok = kb.load(pm_d, [128, 128], name="perm")
    ident, idtok = kb.load(id_d, [128, 128], name="ident")

    for l in range(depth):
        src = xT_d if l == 0 else x_s
        dst = out_d if l == depth - 1 else x_s
        phase_a(kb, l, src, w_in_d[l], gmix_d[l], qg_d[l], rc_d, rs_d, pm, pmtok, vg_d[l],
                qk_s, V_s, u_s, zu_s, vn_s, g_s)
        phase_ssm(kb, l, u_s, y_s, lre_d[l], lim_d[l], ldt_d[l], bt_d[l], ct_d[l], dsk_d[l], iota_d,
                  bg=lambda: attn_gen(kb, qk_s, V_s, at_s))
        moe = (l % 2 == 1)
        phase_c(kb, l, src, x1_s, at_s, y_s, zu_s, vn_s, g_s, glu_d[l], wst_d[l], bs_d[l], wbr_d[l], wout_d[l],
                gffn_d[l], gple_d[l], pg_d[l], pp_d[l], pT_d[l],
                (dw1_d, dw3_d, dw2_d), (rw_d, ew1_d, ew3_d, ew2_d), moe, ident, idtok)
        phase_d(kb, l, x1_s, dst, gffn_d[l], gple_d[l], pg_d[l], pp_d[l], pT_d[l],
                (dw1_d, dw3_d, dw2_d), (rw_d, ew1_d, ew3_d, ew2_d), moe, ident, idtok)
    return kb.done()


def phase_a(kb, l, src, w_in, gmix_d, qg_d, rc_d, rs_d, pm, pmtok, vg_d, qk_s, V_s, u_s, zu_s, vn_s, g_s):
    P = kb.P
    kb.push()
    kb.init_wslots(4, width=256)
    gc, gtok = kb.load(gmix_d, [128, KC], name="gmix")
    qg, qgtok = kb.load(qg_d, [128, 2], name="qg")
    vg, vgtok = kb.load(vg_d, [128, 1024], name="vgain")
    xT = kb.sb([128, KC, TB], F32, "xT")
    xtok = Tok("xT")
    hTs = [kb.sb([128, KC, TB], BF16, "hT%d" % i) for i in range(2)]
    htoks = [Tok("hT%d" % i) for i in range(2)]
    rCs = [kb.sb([128, TB], F32, "ropeC%d" % i) for i in range(2)]
    rSs = [kb.sb([128, TB], F32, "ropeS%d" % i) for i in range(2)]
    rctoks, rstoks = [Tok("rc0"), Tok("rc1")], [Tok("rs0"), Tok("rs1")]
    stg = [kb.sb([128, TB], F32, "stg%d" % i) for i in range(4)]
    stgtok = [Tok("stg%d" % i) for i in range(4)]
    mk = lambda n, dt=F32, w=TB: (kb.sb([128, w], dt, n), Tok(n))
    qf, qftok = mk("qf")
    q2, q2tok = mk("q2")
    qr, qrtok = mk("qr")
    qn, qntok = mk("qn")
    t1, t1tok = mk("t1")
    qo = [mk("qo%d" % i, BF16) for i in range(2)]
    wz = kb.sb([128, KC, 1024], BF16, "wz")
    wztok = Tok("wz")
    wv = kb.sb([128, KC, 256], BF16, "wv")
    wvtok = Tok("wv")
    zg, zgtok = mk("zg", F32, 1024)
    zj, zjtok = mk("zj", F32, 1024)
    ssq = kb.sb([128, 2], F32, "ssq")
    ssqtok = Tok("ssq")
    vno = [mk("vno%d" % i, BF16, 1024) for i in range(2)]
    vo = [mk("vo%d" % i, BF16, 256) for i in range(2)]
    wvv = w_in.rearrange("(kc p) n -> p kc n", p=128)
    for q in range(0, KC, 8):
        P.dma("pool", wv[:, q:q + 8, :], wvv[:, q:q + 8, 1280:1536], wvtok, writes=[wvtok])
    for q in range(0, KC, 4):
        P.dma("pool", wz[:, q:q + 4, :], wvv[:, q:q + 4, 3584:4608], wztok, writes=[wztok])
    cnt = {"s": 0}
    xv = src.rearrange("(kc p) n -> p kc n", p=128)
    for nb2 in range(NBLK // 2):
        c0s = [(nb2 * 2 + sb) * TB for sb in range(2)]
        for sb in range(2):
            c0 = c0s[sb]
            for q in range(0, KC, 8):
                P.dma("sp", xT[:, q:q + 8, :], xv[:, q:q + 8, c0:c0 + TB], xtok, writes=[xtok])
            P.dma("sp", rCs[sb][:, :], rc_d[:, c0:c0 + TB], rctoks[sb], writes=[rctoks[sb]])
            P.dma("sp", rSs[sb][:, :], rs_d[:, c0:c0 + TB], rstoks[sb], writes=[rstoks[sb]])
            kb.rmsnorm(xT, xtok, gc, gtok, hTs[sb], htoks[sb], bank=7)

        def epi_qk(j, b, sb):
            c0 = c0s[sb]
            rC, rS, rctok, rstok_ = rCs[sb], rSs[sb], rctoks[sb], rstoks[sb]
            bk, bt = kb.bank[b], kb.btok[b]
            gi = 0 if j < 8 else 1
            P.op("act", ACTF(qf[:, :], bk[:, :], AF.Identity), reads=[bt], writes=[qftok])
            P.op("act", ACTF(q2[:, :], bk[:, :], AF.Square), reads=[bt], writes=[q2tok])
            P.op("pe", MM(kb.bank[6][:, :], kb.ones32[:, :], q2[:, :]), reads=[q2tok, kb.ones_tok], writes=[kb.btok[6]])
            P.op("act", ACTF(qr[:, :], kb.bank[6][:, :], AF.Sqrt, scale=1.0 / 128, bias=kb.eps_t[:, 0:1]),
                 reads=[kb.btok[6], kb.eps_tok], writes=[qrtok])
            P.op("dve", RCP(qr[:, :], qr[:, :]), reads=[qrtok], writes=[qrtok])
            P.op("dve", STT(qn[:, :], qf[:, :], qg[:, gi:gi + 1], qr[:, :], ALU.mult, ALU.mult),
                 reads=[qftok, qgtok, qrtok], writes=[qntok])
            P.op("pe", MM(kb.bank[6][:, :], pm[:, :], qn[:, :]), reads=[qntok, pmtok], writes=[kb.btok[6]])
            P.op("dve", TT(t1[:, :], qn[:, :], rC[:, :], ALU.mult), reads=[qntok, rctok], writes=[t1tok])
            P.op("dve", TT(qn[:, :], kb.bank[6][:, :], rS[:, :], ALU.mult), reads=[kb.btok[6], rstok_], writes=[qntok])
            o, otok = qo[j % 2]
            P.op("dve", TT(o[:, :], t1[:, :], qn[:, :], ALU.add), reads=[t1tok, qntok], writes=[otok])
            P.dma("sp", qk_s[j * 128:(j + 1) * 128, c0:c0 + TB], o[:, :], otok, reads=[otok])

        def mk_epi_raw(dst_s, j0):
            def epi(j, b, sb):
                c0 = c0s[sb]
                s = cnt["s"] % 4
                cnt["s"] += 1
                if cnt["s"] % 2 == 0:
                    P.op("act", ACTF(stg[s][:, :], kb.bank[b][:, :], AF.Identity), reads=[kb.btok[b]], writes=[stgtok[s]])
                else:
                    P.op("dve", CP(stg[s][:, :], kb.bank[b][:, :]), reads=[kb.btok[b]], writes=[stgtok[s]])
                r0 = (j - j0) * 128
                P.dma("sp", dst_s[r0:r0 + 128, c0:c0 + TB], stg[s][:, :], stgtok[s], reads=[stgtok[s]])
            return epi

        rhs = lambda kc, sb: hTs[sb][:, kc, :]
        kb.linear2(w_in, KC, 0, 1280, rhs, htoks, epi_qk, [0, 1, 2, 3], 2)
        kb.linear2(w_in, KC, 1536, 1024, rhs, htoks, mk_epi_raw(u_s, 12), [0, 1, 2, 3], 2)
        kb.linear2(w_in, KC, 2560, 1024, rhs, htoks, mk_epi_raw(zu_s, 20), [0, 1, 2, 3], 2)
        kb.linear2(w_in, KC, 4608, 6144, rhs, htoks, mk_epi_raw(g_s, 36), [0, 1, 2, 3], 2)
        for tl8 in range(2 * TB // 128):
            sb, tl = tl8 // 4, tl8 % 4
            hT, htok, c0 = hTs[sb], htoks[sb], c0s[sb]
            tsl = slice(tl * 128, (tl + 1) * 128)
            r0 = c0 + tl * 128
            b = 4
            for kc in range(KC):
                P.op("pe", MM(kb.bank[b][:, 0:256], hT[:, kc, tsl], wv[:, kc, :], start=(kc == 0), stop=(kc == KC - 1)),
                     reads=[htok, wvtok], writes=[kb.btok[b]])
            o, otok = vo[tl % 2]
            P.op("dve", CP(o[:, :], kb.bank[b][:, 0:256]), reads=[kb.btok[b]], writes=[otok])
            P.dma("sp", V_s[r0:r0 + 128, :], o[:, :], otok, reads=[otok])
            for half in range(2):
                b = 5 + half
                for kc in range(KC):
                    P.op("pe", MM(kb.bank[b][:, :], hT[:, kc, tsl], wz[:, kc, half * 512:(half + 1) * 512],
                                  start=(kc == 0), stop=(kc == KC - 1)),
                         reads=[htok, wztok], writes=[kb.btok[b]])
                P.op("act", ACTF(zg[:, half * 512:(half + 1) * 512], kb.bank[b][:, :], AF.Gelu_apprx_tanh),
                     reads=[kb.btok[b]], writes=[zgtok])
            P.op("dve", TT(zj[:, :], zg[:, :], zg[:, :], ALU.mult), reads=[zgtok], writes=[zjtok])
            P.op("dve", lambda e: e.reduce_sum(ssq[:, 0:1], zj[:, :], AX.X), reads=[zjtok], writes=[ssqtok])
            P.op("act", ACTF(ssq[:, 1:2], ssq[:, 0:1], AF.Sqrt, scale=1.0 / 1024, bias=kb.eps_t[:, 0:1]),
                 reads=[ssqtok, kb.eps_tok], writes=[ssqtok])
            P.op("dve", RCP(ssq[:, 1:2], ssq[:, 1:2]), reads=[ssqtok], writes=[ssqtok])
            o, otok = vno[tl % 2]
            P.op("dve", STT(o[:, :], zg[:, :], ssq[:, 1:2], vg[:, :], ALU.mult, ALU.mult),
                 reads=[zgtok, ssqtok, vgtok], writes=[otok])
            P.dma("sp", vn_s[r0:r0 + 128, :], o[:, :], otok, reads=[otok])
    kb.pop()


def attn_gen(kb, qk_s, V_s, at_s):
    P = kb.P
    kT = kb.sb([128, 2, SEQ], BF16, "kT")
    ktok = Tok("kT")
    kv = qk_s[1024:1280, :].rearrange("(g p) n -> p g n", p=128)
    for g in range(2):
        P.dma("sp", kT[:, g, :], kv[:, g, :], ktok, writes=[ktok])
    V = kb.sb([128, 32, 256], BF16, "V")
    vtok = Tok("V")
    vv = V_s.rearrange("(kb p) c -> p kb c", p=128)
    for q in range(0, 32, 8):
        P.dma("sp", V[:, q:q + 8, :], vv[:, q:q + 8, :], vtok, writes=[vtok])
    qt = [kb.sb([128, 8, TB], BF16, "q%d" % i) for i in range(2)]
    qtok = [Tok("q%d" % i) for i in range(2)]
    pt = [kb.sb([128, TB], BF16, "pt%d" % i) for i in range(3)]
    pttok = [Tok("pt%d" % i) for i in range(3)]
    rd = kb.sb([128, TB], F32, "rden")
    rdtok = Tok("rden")
    ost = [kb.sb([128, 8, TB], BF16, "ost%d" % i) for i in range(2)]
    ostok = [Tok("ost%d" % i) for i in range(2)]
    scale = 128 ** -0.5
    qv = qk_s[0:1024, :].rearrange("(h p) n -> p h n", p=128)
    av = at_s.rearrange("(h p) n -> p h n", p=128)
    it = 0
    for qb in range(NBLK):
        c0 = qb * TB
        q, qtk = qt[qb % 2], qtok[qb % 2]
        P.dma("sp", q[:, :, :], qv[:, :, c0:c0 + TB], qtk, writes=[qtk])
        os_, ostk = ost[qb % 2], ostok[qb % 2]
        for h in range(8):
            g = h // 4
            ob, db = 5, 6
            def emit_S(kbk_, sbk_):
                P.op("pe", MM(kb.bank[3 + sbk_][:, :], kT[:, g, kbk_ * 128:(kbk_ + 1) * 128], q[:, h, :]),
                     reads=[ktok, qtk], writes=[kb.btok[3 + sbk_]])

            emit_S(0, it % 2)
            for kbk in range(32):
                sbk = it % 2
                it += 1
                sbn = 3 + sbk
                P.op("act", ACTF(pt[sbk][:, :], kb.bank[sbn][:, :], AF.Exp, scale=scale),
                     reads=[kb.btok[sbn]], writes=[pttok[sbk]])
                if kbk + 1 < 32:
                    emit_S(kbk + 1, it % 2)
                P.op("pe", MM(kb.bank[ob][:, :], V[:, kbk, g * 128:(g + 1) * 128], pt[sbk][:, :],
                              start=(kbk == 0), stop=(kbk == 31)),
                     reads=[vtok, pttok[sbk]], writes=[kb.btok[ob]])
                P.op("pe", MM(kb.bank[db][:, :], kb.onesb[:, :], pt[sbk][:, :], start=(kbk == 0), stop=(kbk == 31)),
                     reads=[kb.onesb_tok, pttok[sbk]], writes=[kb.btok[db]])
                yield
            P.op("dve", RCP(rd[:, :], kb.bank[db][:, :]), reads=[kb.btok[db]], writes=[rdtok])
            P.op("dve", TT(os_[:, h, :], kb.bank[ob][:, :], rd[:, :], ALU.mult), reads=[kb.btok[ob], rdtok], writes=[ostk])
        P.dma("sp", av[:, :, c0:c0 + TB], os_[:, :, :], ostk, reads=[ostk])


MAGIC = 12582912.0
TWO_PI = float(2 * np.pi)


def phase_ssm(kb, l, u_s, y_s, lre_d, lim_d, ldt_d, bt_d, ct_d, dsk_d, iota_d, bg=None):
    P = kb.P
    kb.push()
    bgs = [bg() if bg is not None else None]

    def tick(n):
        for _ in range(n):
            if bgs[0] is None:
                return
            try:
                next(bgs[0])
            except StopIteration:
                bgs[0] = None

    NP = 64
    ld = lambda d, n: kb.load(d, [128, NP], name=n)
    lre, t_lre = ld(lre_d, "lre")
    lim, t_lim = ld(lim_d, "lim")
    ldt, t_ldt = ld(ldt_d, "ldt")
    dsk, t_dsk = kb.load(dsk_d, [128, 8], name="dsk")
    iota, t_iota = kb.load(iota_d, [128, 2, 512], name="iota")
    tk = Tok("ssmpre")
    mk = lambda n: kb.sb([128, NP], F32, n)
    dt_, r_, th, kk, sn, cs, a_re, a_im, den, k_re, k_im, tmp, cT, sT, thT = [mk("p%d" % i) for i in range(15)]
    pre_reads = [t_lre, t_lim, t_ldt, tk]

    def op(fn, eng="dve"):
        P.op(eng, fn, reads=pre_reads, writes=[tk])

    def sincos(ang, s_out, c_out):
        op(TS(kk[:, :], ang[:, :], 1.0 / TWO_PI, MAGIC, ALU.mult, ALU.add))
        op(TS(kk[:, :], kk[:, :], MAGIC, None, ALU.subtract))
        op(STT(tmp[:, :], kk[:, :], -TWO_PI, ang[:, :], ALU.mult, ALU.add))
        op(ACTF(s_out[:, :], tmp[:, :], AF.Sin), "act")
        op(TS(tmp[:, :], ang[:, :], float(np.pi / 2), None, ALU.add))
        op(TS(kk[:, :], tmp[:, :], 1.0 / TWO_PI, MAGIC, ALU.mult, ALU.add))
        op(TS(kk[:, :], kk[:, :], MAGIC, None, ALU.subtract))
        op(STT(tmp[:, :], kk[:, :], -TWO_PI, tmp[:, :], ALU.mult, ALU.add))
        op(ACTF(c_out[:, :], tmp[:, :], AF.Sin), "act")

    op(ACTF(dt_[:, :], ldt[:, :], AF.Exp), "act")
    op(TT(r_[:, :], lre[:, :], dt_[:, :], ALU.mult))
    op(ACTF(r_[:, :], r_[:, :], AF.Exp), "act")
    op(TT(th[:, :], lim[:, :], dt_[:, :], ALU.mult))
    sincos(th, sn, cs)
    op(TT(a_re[:, :], r_[:, :], cs[:, :], ALU.mult))
    op(TT(a_im[:, :], r_[:, :], sn[:, :], ALU.mult))
    op(TS(a_re[:, :], a_re[:, :], -1.0, None, ALU.add))
    op(TT(den[:, :], lre[:, :], lre[:, :], ALU.mult))
    op(TT(tmp[:, :], lim[:, :], lim[:, :], ALU.mult))
    op(TT(den[:, :], den[:, :], tmp[:, :], ALU.add))
    op(RCP(den[:, :], den[:, :]))
    op(TT(k_re[:, :], a_re[:, :], lre[:, :], ALU.mult))
    op(TT(tmp[:, :], a_im[:, :], lim[:, :], ALU.mult))
    op(TT(k_re[:, :], k_re[:, :], tmp[:, :], ALU.add))
    op(TT(k_re[:, :], k_re[:, :], den[:, :], ALU.mult))
    op(TT(k_im[:, :], a_im[:, :], lre[:, :], ALU.mult))
    op(TT(tmp[:, :], a_re[:, :], lim[:, :], ALU.mult))
    op(TT(k_im[:, :], k_im[:, :], tmp[:, :], ALU.subtract))
    op(TT(k_im[:, :], k_im[:, :], den[:, :], ALU.mult))
    op(TS(thT[:, :], th[:, :], 512.0, None, ALU.mult))
    sincos(thT, sT, cT)

    mkw = lambda n, dt=F32, w=512: (kb.sb([128, w], dt, n), Tok(n))
    Ec, t_Ec = mkw("Ec")
    Es, t_Es = mkw("Es")
    Dr, t_Dr = mkw("Dr")
    Di, t_Di = mkw("Di")
    ph, t_ph = mkw("ph")
    kq, t_kq = mkw("kq")
    m1, t_m1 = mkw("m1")
    m2, t_m2 = mkw("m2")
    m3, t_m3 = mkw("m3")
    m4, t_m4 = mkw("m4")
    t_ini2, t_ini3 = Tok("ini2"), Tok("ini3")
    zr, t_zr = mkw("zr")
    zi, t_zi = mkw("zi")
    sr = [mkw("sr%d" % i) for i in range(2)]
    si = [mkw("si%d" % i) for i in range(2)]
    Pp = [mkw("P%d" % i, BF16) for i in range(4)]
    ini, t_ini = kb.sb([128, 4], F32, "ini"), Tok("ini")
    BTr, t_BT = kb.sb([128, 2, 128], BF16, "BTs"), Tok("BTs")
    Cf, t_Cf = kb.sb([128, 2, 128], F32, "Cf"), Tok("Cf")
    Cb, t_Cb = kb.sb([128, 3, 128], BF16, "Cb"), Tok("Cb")
    ub, t_ub = kb.sb([128, SEQ], BF16, "ub"), Tok("ub")
    uf, t_uf = kb.sb([128, SEQ], F32, "uf"), Tok("uf")
    ysb, t_y = kb.sb([128, SEQ], F32, "ysb"), Tok("ysb")

    def table(ang_col, iota_ap, s_out, t_s, c_out, t_c):
        P.op("dve", TS(ph[:, :], iota_ap, ang_col, None, ALU.mult), reads=[t_iota, tk], writes=[t_ph])
        P.op("dve", TS(kq[:, :], ph[:, :], 1.0 / TWO_PI, MAGIC, ALU.mult, ALU.add), reads=[t_ph], writes=[t_kq])
        P.op("dve", TS(kq[:, :], kq[:, :], MAGIC, None, ALU.subtract), reads=[t_kq], writes=[t_kq])
        P.op("dve", STT(m1[:, :], kq[:, :], -TWO_PI, ph[:, :], ALU.mult, ALU.add), reads=[t_kq, t_ph], writes=[t_m1])
        P.op("act", ACTF(s_out[:, :], m1[:, :], AF.Sin), reads=[t_m1], writes=[t_s])
        P.op("dve", TS(ph[:, :], ph[:, :], float(np.pi / 2), None, ALU.add), reads=[t_ph], writes=[t_ph])
        P.op("dve", TS(kq[:, :], ph[:, :], 1.0 / TWO_PI, MAGIC, ALU.mult, ALU.add), reads=[t_ph], writes=[t_kq])
        P.op("dve", TS(kq[:, :], kq[:, :], MAGIC, None, ALU.subtract), reads=[t_kq], writes=[t_kq])
        P.op("dve", STT(m1[:, :], kq[:, :], -TWO_PI, ph[:, :], ALU.mult, ALU.add), reads=[t_kq, t_ph], writes=[t_m1])
        P.op("act", ACTF(c_out[:, :], m1[:, :], AF.Sin), reads=[t_m1], writes=[t_c])

    for cb in range(8):
        P.dma("pool", ub[:, :], u_s[cb * 128:(cb + 1) * 128, :], t_ub, writes=[t_ub])
        P.dma("sp", uf[:, :], u_s[cb * 128:(cb + 1) * 128, :], t_uf, writes=[t_uf])
        first = True
        for d in range(2):
            for jb in range(4):
                pd = d * 32 + cb * 4 + jb
                col = slice(pd, pd + 1)
                io = iota[:, d, :]
                table(th[:, col], io, Es, t_Es, Ec, t_Ec)
                P.op("dve", TS(m1[:, :], Es[:, :], k_im[:, col], None, ALU.mult), reads=[t_Es, tk], writes=[t_m1])
                P.op("dve", STT(Dr[:, :], Ec[:, :], k_re[:, col], m1[:, :], ALU.mult, ALU.add), reads=[t_Ec, tk, t_m1], writes=[t_Dr])
                P.op("dve", TS(m1[:, :], Es[:, :], k_re[:, col], None, ALU.mult), reads=[t_Es, tk], writes=[t_m1])
                P.op("dve", STT(Di[:, :], Ec[:, :], k_im[:, col], m1[:, :], ALU.mult, ALU.subtract), reads=[t_Ec, tk, t_m1], writes=[t_Di])
                P.dma("pool", BTr[:, :, :], bt_d[pd].rearrange("r p c -> p r c"), t_BT, writes=[t_BT])
                P.dma("sp", Cf[:, :, :], ct_d[pd].rearrange("r p c -> p r c"), t_Cf, writes=[t_Cf])
                P.op("dve", CP(Cb[:, 0, :], Cf[:, 0, :]), reads=[t_Cf], writes=[t_Cb])
                P.op("dve", TS(Cb[:, 1, :], Cf[:, 0, :], -1.0, None, ALU.mult), reads=[t_Cf], writes=[t_Cb])
                P.op("dve", TS(Cb[:, 2, :], Cf[:, 1, :], -1.0, None, ALU.mult), reads=[t_Cf], writes=[t_Cb])
                chunks = list(range(8)) if d == 0 else list(range(7, -1, -1))
                prev = None
                pend = [None]
                b0, b1 = kb.bank[0], kb.bank[1]

                def emit_B(ch_):
                    c_ = slice(ch_ * 512, (ch_ + 1) * 512)
                    P.op("pe", MM(kb.bank[0][:, :], BTr[:, 0, :], ub[:, c_]), reads=[t_BT, t_ub], writes=[kb.btok[0]])
                    P.op("pe", MM(kb.bank[1][:, :], BTr[:, 1, :], ub[:, c_]), reads=[t_BT, t_ub], writes=[kb.btok[1]])

                emit_B(chunks[0])
                for ci, ch in enumerate(chunks):
                    cs_ = slice(ch * 512, (ch + 1) * 512)
                    P.op("dve", TT(m1[:, :], b0[:, :], Dr[:, :], ALU.mult), reads=[kb.btok[0], t_Dr], writes=[t_m1])
                    P.op("dve", TT(m2[:, :], b1[:, :], Di[:, :], ALU.mult), reads=[kb.btok[1], t_Di], writes=[t_m2])
                    P.op("dve", TT(m3[:, :], b0[:, :], Di[:, :], ALU.mult), reads=[kb.btok[0], t_Di], writes=[t_m3])
                    P.op("dve", TT(m4[:, :], b1[:, :], Dr[:, :], ALU.mult), reads=[kb.btok[1], t_Dr], writes=[t_m4])
                    if ci + 1 < len(chunks):
                        emit_B(chunks[ci + 1])
                    tick(4)
                    P.op("pool", TT(zr[:, :], m1[:, :], m2[:, :], ALU.subtract), reads=[t_m1, t_m2], writes=[t_zr])
                    P.op("pool", TT(zi[:, :], m3[:, :], m4[:, :], ALU.add), reads=[t_m3, t_m4], writes=[t_zi])
                    if pend[0] is not None:
                        pend[0]()
                        pend[0] = None
                    (srt, t_sr), (sit, t_si) = sr[ci % 2], si[ci % 2]
                    if prev is None:
                        init_r, init_i = 0.0, 0.0
                        ireads = []
                    else:
                        (pr_, t_pr), (pi_, t_pi) = prev
                        e_r = pr_[:, 511:512] if d == 0 else pr_[:, 0:1]
                        e_i = pi_[:, 511:512] if d == 0 else pi_[:, 0:1]
                        P.op("dve", TS(ini[:, 2:3], e_i, sT[:, col], None, ALU.mult), reads=[t_pi, tk], writes=[t_ini2])
                        P.op("dve", TS(ini[:, 3:4], e_i, cT[:, col], None, ALU.mult), reads=[t_pi, tk], writes=[t_ini3])
                        P.op("dve", STT(ini[:, 0:1], e_r, cT[:, col], ini[:, 2:3], ALU.mult, ALU.subtract),
                             reads=[t_pr, tk, t_ini2], writes=[t_ini])
                        P.op("dve", STT(ini[:, 1:2], e_r, sT[:, col], ini[:, 3:4], ALU.mult, ALU.add),
                             reads=[t_pr, tk, t_ini3], writes=[t_ini])
                        init_r, init_i = ini[:, 0:1], ini[:, 1:2]
                        ireads = [t_ini]
                    rb = r_[:, col].broadcast_to([128, 512])
                    if d == 0:
                        o_r, o_i, i_r, i_i = srt[:, :], sit[:, :], zr[:, :], zi[:, :]
                    else:
                        o_r, o_i = rev_view(srt[:, :], 512), rev_view(sit[:, :], 512)
                        i_r, i_i = rev_view(zr[:, :], 512), rev_view(zi[:, :], 512)
                    P.op("dve", (lambda o, dd, ii, it_: (lambda e: e.tensor_tensor_scan(o, dd, ii, it_, ALU.mult, ALU.add)))(o_r, rb, i_r, init_r),
                         reads=[tk, t_zr] + ireads, writes=[t_sr])
                    P.op("dve", (lambda o, dd, ii, it_: (lambda e: e.tensor_tensor_scan(o, dd, ii, it_, ALU.mult, ALU.add)))(o_i, rb, i_i, init_i),
                         reads=[tk, t_zi] + ireads, writes=[t_si])
                    prev = ((srt, t_sr), (sit, t_si))
                    for (pi4, a_, ta_, b_, tb_) in ((0, srt, t_sr, Ec, t_Ec), (1, sit, t_si, Es, t_Es),
                                                    (2, sit, t_si, Ec, t_Ec), (3, srt, t_sr, Es, t_Es)):
                        P.op("pool", TT(Pp[pi4][0][:, :], a_[:, :], b_[:, :], ALU.mult), reads=[ta_, tb_], writes=[Pp[pi4][1]])
                    for (pi4, ci3) in ((0, 0), (1, 1), (2, 2), (3, 2)):
                        P.op("pe", MM(kb.bank[2][:, :], Cb[:, ci3, :], Pp[pi4][0][:, :], start=(pi4 == 0), stop=(pi4 == 3)),
                             reads=[t_Cb, Pp[pi4][1]], writes=[kb.btok[2]])
                    def yacc(cs_=cs_, first=first):
                        if first:
                            P.op("dve", STT(ysb[:, cs_], uf[:, cs_], dsk[:, cb:cb + 1], kb.bank[2][:, :], ALU.mult, ALU.add),
                                 reads=[t_uf, t_dsk, kb.btok[2]], writes=[t_y])
                        else:
                            P.op("dve", TT(ysb[:, cs_], kb.bank[2][:, :], ysb[:, cs_], ALU.add), reads=[kb.btok[2], t_y], writes=[t_y])
                    pend[0] = yacc
                if pend[0] is not None:
                    pend[0]()
                    pend[0] = None
                first = False
        P.dma("sp", y_s[cb * 128:(cb + 1) * 128, :], ysb[:, :], t_y, reads=[t_y])
    tick(1 << 30)
    kb.pop()


def phase_c(kb, l, src, x1_s, at_s, y_s, zu_s, vn_s, g_s, glu_d, wst_d, bs_d, wbr_d, wout_d,
            gffn_d, gple_d, pg_d, pp_d, pT_d, dense, moe_w, moe, ident, idtok):
    P = kb.P
    kb.push()
    kb.init_wslots(3)
    gf, t_gf = kb.load(gffn_d, [128, KC], name="gffn")
    gp, t_gp = kb.load(gple_d, [128, KC], name="gple")
    wsT, t_ws = kb.load(wst_d, [128, 8, 128], BF16, eng="pool", name="wsT")
    bsr, t_bs = kb.load(bs_d, [1, 1024], BF16, eng="pool", name="bsrow")
    xT, t_x = kb.sb([128, KC, TB], F32, "xT"), Tok("xT")
    hT, t_h = kb.sb([128, KC, TB], BF16, "hT"), Tok("hT")
    mkw = lambda n, dt=F32: (kb.sb([128, TB], dt, n), Tok(n))
    tm = [mkw("tm%d" % i) for i in range(3)]
    ma, t_ma = mkw("ma")
    mb, t_mb = mkw("mb")
    cnt = {"t": 0}
    xv = src.rearrange("(kc p) n -> p kc n", p=128)

    def residual_epi(j, b):
        P.op("dve", TT(xT[:, j, :], kb.bank[b][:, :], xT[:, j, :], ALU.add), reads=[kb.btok[b], t_x], writes=[t_x])

    for nb in range(NBLK):
        c0 = nb * TB
        csl = slice(c0, c0 + TB)
        for q in range(0, KC, 8):
            P.dma("sp", xT[:, q:q + 8, :], xv[:, q:q + 8, csl], t_x, writes=[t_x])
        kb.push()
        br, t_br = kb.sb([128, 24, TB], BF16, "br"), [Tok("br%d" % i) for i in range(3)]
        gt = [(kb.sb([128, 3, TB], F32, "gt%d" % i), Tok("gt%d" % i)) for i in range(2)]
        kb.push()
        y32, t_y32 = kb.sb([128, 8, TB], F32, "y32"), Tok("y32")
        yb, t_yb = kb.sb([128, 8, TB], BF16, "yb"), Tok("yb")
        P.dma("sp", y32[:, :, :], y_s.rearrange("(kc p) n -> p kc n", p=128)[:, :, csl], t_y32, writes=[t_y32])
        for kc in range(8):
            P.op("act", ACTF(y32[:, kc, :], y32[:, kc, :], AF.Gelu_apprx_tanh), reads=[t_y32], writes=[t_y32])
            P.op("dve", CP(yb[:, kc, :], y32[:, kc, :]), reads=[t_y32], writes=[t_yb])

        def epi_glu(j, b):
            t, tt = tm[cnt["t"] % 3]
            cnt["t"] += 1
            P.op("act", ACTF(t[:, :], kb.bank[b][:, :], AF.Sigmoid), reads=[kb.btok[b]], writes=[tt])
            P.op("dve", TT(br[:, 8 + j, :], y32[:, j, :], t[:, :], ALU.mult), reads=[t_y32, tt], writes=[t_br[1]])

        kb.linear(glu_d, 8, 0, 1024, lambda kc: yb[:, kc, :], [t_yb], epi_glu, banks=[0, 1, 2, 3])
        kb.pop()
        P.dma("sp", br[:, 0:8, :], at_s.rearrange("(kc p) n -> p kc n", p=128)[:, :, csl], t_br[0], writes=[t_br[0]])
        kb.push()
        zu, t_zu = kb.sb([128, 8, TB], F32, "zu"), Tok("zu")
        vn, t_vn = kb.sb([128, 4, 1024], BF16, "vn"), Tok("vn")
        P.dma("sp", zu[:, :, :], zu_s.rearrange("(kc p) n -> p kc n", p=128)[:, :, csl], t_zu, writes=[t_zu])
        P.dma("sp", vn[:, :, :], vn_s[c0:c0 + TB, :].rearrange("(t p) c -> p t c", p=128), t_vn, writes=[t_vn])
        for kc in range(8):
            P.op("act", ACTF(zu[:, kc, :], zu[:, kc, :], AF.Gelu_apprx_tanh), reads=[t_zu], writes=[t_zu])
        for g in range(8):
            b = g % 4
            for tl in range(4):
                osl = slice(tl * 128, (tl + 1) * 128)
                P.op("pe", MM(kb.bank[b][:, osl], vn[:, tl, g * 128:(g + 1) * 128], wsT[:, g, :], start=True, stop=False),
                     reads=[t_vn, t_ws], writes=[kb.btok[b]])
                P.op("pe", MM(kb.bank[b][:, osl], kb.onesb[0:1, :], bsr[0:1, g * 128:(g + 1) * 128], start=False, stop=True),
                     reads=[kb.onesb_tok, t_bs], writes=[kb.btok[b]])
            P.op("dve", TT(br[:, 16 + g, :], kb.bank[b][:, :], zu[:, g, :], ALU.mult), reads=[kb.btok[b], t_zu], writes=[t_br[2]])
        kb.pop()
        bi = 0
        gview = g_s.rearrange("(n j p) t -> j p n t", n=3, p=128)
        for g0 in range(0, D, WS_W):
            wts = [kb.load_w(wbr_d[n * 1024:(n + 1) * 1024, :], 8, g0, WS_W) for n in range(3)]
            for jj in range(WS_W // 128):
                j = (g0 + jj * 128) // 128
                gtt, t_gt = gt[j % 2]
                P.dma("sp", gtt[:, :, :], gview[j][:, :, csl], t_gt, writes=[t_gt])
                for n in range(3):
                    P.op("act", ACTF(gtt[:, n, :], gtt[:, n, :], AF.Sigmoid), reads=[t_gt], writes=[t_gt])
                banks = [(bi * 3 + n) % 6 for n in range(3)]
                bi += 1
                for n in range(3):
                    for k in range(8):
                        P.op("pe", MM(kb.bank[banks[n]][:, :], wts[n][0][:, k, jj * 128:(jj + 1) * 128], br[:, n * 8 + k, :],
                                      start=(k == 0), stop=(k == 7)),
                             reads=[wts[n][1], t_br[n]], writes=[kb.btok[banks[n]]])
                P.op("dve", TT(ma[:, :], kb.bank[banks[0]][:, :], gtt[:, 0, :], ALU.mult), reads=[kb.btok[banks[0]], t_gt], writes=[t_ma])
                P.op("dve", TT(mb[:, :], kb.bank[banks[1]][:, :], gtt[:, 1, :], ALU.mult), reads=[kb.btok[banks[1]], t_gt], writes=[t_mb])
                P.op("dve", TT(ma[:, :], ma[:, :], mb[:, :], ALU.add), reads=[t_ma, t_mb], writes=[t_ma])
                P.op("dve", TT(mb[:, :], kb.bank[banks[2]][:, :], gtt[:, 2, :], ALU.mult), reads=[kb.btok[banks[2]], t_gt], writes=[t_mb])
                P.op("dve", TT(hT[:, j, :], ma[:, :], mb[:, :], ALU.add), reads=[t_ma, t_mb], writes=[t_h])
        kb.pop()
        kb.linear(wout_d, KC, 0, D, lambda kc: hT[:, kc, :], [t_h], residual_epi, banks=[0, 1, 2, 3])
        if kb.dbg is not None and moe and nb == 0:
            P.dma("sp", kb.dbg[0].rearrange("(kc p) n -> p kc n", p=128), xT[:, :, :], t_x, reads=[t_x], final=True)
        x1v = x1_s.rearrange("(kc p) n -> p kc n", p=128)
        for q in range(0, KC, 8):
            P.dma("sp", x1v[:, q:q + 8, csl], xT[:, q:q + 8, :], t_x, reads=[t_x])
    kb.pop()


SB2 = 2


def phase_d(kb, l, x1_s, dst, gffn_d, gple_d, pg_d, pp_d, pT_d, dense, moe_w, moe, ident, idtok):
    P = kb.P
    kb.push()
    kb.init_wslots(4, width=256)
    gf, t_gf = kb.load(gffn_d, [128, KC], name="gffn")
    gp, t_gp = kb.load(gple_d, [128, KC], name="gple")
    if moe:
        rwh, t_rwh = kb.sb([128, KC, 128], BF16, "rwh"), Tok("rwh")
        rwl, t_rwl = kb.sb([128, KC, 128], BF16, "rwl"), Tok("rwl")
        kb.push()
        rw, t_rw = kb.load(moe_w[0], [128, KC, 128], name="rw")
        P.op("dve", CP(rwh[:, :, :], rw[:, :, :]), reads=[t_rw], writes=[t_rwh])
        P.op("dve", TT(rwl[:, :, :], rw[:, :, :], rwh[:, :, :], ALU.subtract), reads=[t_rw, t_rwh], writes=[t_rwl])
        kb.pop()
    xT = [kb.sb([128, KC, TB], F32, "xT%d" % i) for i in range(SB2)]
    t_x = [Tok("xT%d" % i) for i in range(SB2)]
    hT = [kb.sb([128, KC, TB], BF16, "hT%d" % i) for i in range(SB2)]
    t_h = [Tok("hT%d" % i) for i in range(SB2)]
    mkw = lambda n, dt=F32: (kb.sb([128, TB], dt, n), Tok(n))
    tm = [mkw("tm%d" % i) for i in range(4)]
    cnt = {"t": 0}
    xv = x1_s.rearrange("(kc p) n -> p kc n", p=128)
    dv = dst.rearrange("(kc p) n -> p kc n", p=128)

    def mk_res(sb):
        def residual_epi(j, b):
            P.op("dve", TT(xT[sb][:, j, :], kb.bank[b][:, :], xT[sb][:, j, :], ALU.add), reads=[kb.btok[b], t_x[sb]], writes=[t_x[sb]])
        return residual_epi

    for nb2 in range(NBLK // SB2):
        for sb in range(SB2):
            c0 = (nb2 * SB2 + sb) * TB
            for q in range(0, KC, 8):
                P.dma("sp", xT[sb][:, q:q + 8, :], xv[:, q:q + 8, c0:c0 + TB], t_x[sb], writes=[t_x[sb]])
        kb.push()
        act = [kb.sb([128, 8, TB], BF16, "act%d" % i) for i in range(SB2)]
        t_act = [Tok("act%d" % i) for i in range(SB2)]
        if moe:
            h32 = [mkw("h32_%d" % i) for i in range(2)]
            hlo = [mkw("hlo_%d" % i, BF16) for i in range(2)]
            lg, t_lg = kb.sb([128, 4, 8], F32, "lg"), Tok("lg")
            t8, t_t8 = kb.sb([128, 8], F32, "t8"), Tok("t8")
            sm, t_sm = kb.sb([128, 8], F32, "sm"), Tok("sm")
            ex, t_ex = kb.sb([128, 8], F32, "ex"), Tok("ex")
            mask, t_mask = kb.sb([128, 8], F32, "mask"), Tok("mask")
            wg, t_wg = kb.sb([128, 4, 8], F32, "wg"), Tok("wg")
            wgb, t_wgb = kb.sb([128, 128], F32, "wgb"), Tok("wgb")
            gE = [kb.sb([128, 8, TB], BF16, "gE%d" % i) for i in range(SB2)]
            t_gE = [Tok("gE%d" % i) for i in range(SB2)]
        for sb in range(SB2):
            if not moe:
                kb.rmsnorm(xT[sb], t_x[sb], gf, t_gf, hT[sb], t_h[sb], bank=7)
                continue

            def h32cb(kc, sb=sb):
                hh, t_hh = h32[kc % 2]
                P.op("dve", STT(hh[:, :], xT[sb][:, kc, :], gf[:, kc:kc + 1], kb.rstd[:, :], ALU.mult, ALU.mult),
                     reads=[t_x[sb], t_gf, kb.rstok], writes=[t_hh])
                hl, t_hl = hlo[kc % 2]
                P.op("dve", TT(hl[:, :], hh[:, :], hT[sb][:, kc, :], ALU.subtract), reads=[t_hh, t_h[sb]], writes=[t_hl])
                for tl in range(4):
                    tsl = slice(tl * 128, (tl + 1) * 128)
                    o = kb.bank[tl][:, 0:128]
                    P.op("pe", MM(o, hT[sb][:, kc, tsl], rwh[:, kc, :], start=(kc == 0), stop=False),
                         reads=[t_h[sb], t_rwh], writes=[kb.btok[tl]])
                    P.op("pe", MM(o, hl[:, tsl], rwh[:, kc, :], start=False, stop=False),
                         reads=[t_hl, t_rwh], writes=[kb.btok[tl]])
                    P.op("pe", MM(o, hT[sb][:, kc, tsl], rwl[:, kc, :], start=False, stop=(kc == KC - 1)),
                         reads=[t_h[sb], t_rwl], writes=[kb.btok[tl]])

            kb.rmsnorm(xT[sb], t_x[sb], gf, t_gf, hT[sb], t_h[sb], bank=7, h32cb=h32cb)
            for tl in range(4):
                P.op("dve", CP(lg[:, tl, :], kb.bank[tl][:, 0:8]), reads=[kb.btok[tl]], writes=[t_lg])
            for tl in range(4):
                P.op("dve", lambda e, tl=tl: e.max(t8[:, :], lg[:, tl, :]), reads=[t_lg], writes=[t_t8])
                P.op("dve", TS(sm[:, 0:1], t8[:, 0:1], -1.0, None, ALU.mult), reads=[t_t8], writes=[t_sm])
                P.op("dve", TS(mask[:, :], lg[:, tl, :], t8[:, 1:2], None, ALU.is_ge), reads=[t_lg, t_t8], writes=[t_mask])
                P.op("act", ACTF(ex[:, :], lg[:, tl, :], AF.Exp, bias=sm[:, 0:1]), reads=[t_lg, t_sm], writes=[t_ex])
                P.op("act", ACTF(sm[:, 1:2], t8[:, 1:2], AF.Exp, bias=sm[:, 0:1]), reads=[t_t8, t_sm], writes=[t_sm])
                P.op("dve", TS(sm[:, 2:3], sm[:, 1:2], 1.0, None, ALU.add), reads=[t_sm], writes=[t_sm])
                P.op("dve", RCP(sm[:, 2:3], sm[:, 2:3]), reads=[t_sm], writes=[t_sm])
                P.op("dve", STT(wg[:, tl, :], ex[:, :], sm[:, 2:3], mask[:, :], ALU.mult, ALU.mult),
                     reads=[t_ex, t_sm, t_mask], writes=[t_wg])
            for e_ in range(8):
                b = e_ % 2
                for tl in range(4):
                    P.op("dve", CP(wgb[:, :], wg[:, tl, e_:e_ + 1].broadcast_to([128, 128])), reads=[t_wg], writes=[t_wgb])
                    P.op("pe", MM(kb.bank[b][:, tl * 128:(tl + 1) * 128], wgb[:, :], ident[:, :]), reads=[t_wgb, idtok], writes=[kb.btok[b]])
                P.op("act", ACTF(gE[sb][:, e_, :], kb.bank[b][:, :], AF.Identity), reads=[kb.btok[b]], writes=[t_gE[sb]])
        res = [mk_res(sb) for sb in range(SB2)]
        if not moe:
            w1, w3, w2 = dense
            ffn_group_loop(kb, w1, w3, w2, 5632, hT, t_h, act, t_act, tm, cnt, res, None)
        else:
            rw_, ew1, ew3, ew2 = moe_w
            for e_ in range(8):
                ffn_group_loop(kb, ew1[e_], ew3[e_], ew2[e_], 7168, hT, t_h, act, t_act, tm, cnt, res, (gE, e_, t_gE))
        kb.pop()
        kb.push()
        ppw, t_ppw = kb.sb([128, 2, D], BF16, "ppw"), Tok("ppw")
        pTb, t_pT = kb.sb([128, 2, TB], BF16, "pTb"), Tok("pTb")
        P.dma("pool", ppw[:, :, :], pp_d.rearrange("(kc p) n -> p kc n", p=128), t_ppw, writes=[t_ppw])
        for sb in range(SB2):
            c0 = (nb2 * SB2 + sb) * TB
            csl = slice(c0, c0 + TB)
            kb.rmsnorm(xT[sb], t_x[sb], gp, t_gp, hT[sb], t_h[sb], bank=7)
            P.dma("pool", pTb[:, :, :], pT_d.rearrange("(kc p) n -> p kc n", p=128)[:, :, csl], t_pT, writes=[t_pT])

            def epi_ple(j, b, sb=sb):
                t, tt = tm[cnt["t"] % 4]
                cnt["t"] += 1
                P.op("act", ACTF(t[:, :], kb.bank[b][:, :], AF.Sigmoid), reads=[kb.btok[b]], writes=[tt])
                pb = 4 + (j % 2)
                for k in range(2):
                    P.op("pe", MM(kb.bank[pb][:, :], ppw[:, k, j * 128:(j + 1) * 128], pTb[:, k, :], start=(k == 0), stop=(k == 1)),
                         reads=[t_ppw, t_pT], writes=[kb.btok[pb]])
                P.op("dve", TT(t[:, :], kb.bank[pb][:, :], t[:, :], ALU.mult), reads=[kb.btok[pb], tt], writes=[tt])
                P.op("dve", TT(xT[sb][:, j, :], xT[sb][:, j, :], t[:, :], ALU.add), reads=[t_x[sb], tt], writes=[t_x[sb]])

            kb.linear(pg_d, KC, 0, D, lambda kc, sb=sb: hT[sb][:, kc, :], [t_h[sb]], epi_ple, banks=[0, 1, 2, 3])
            for q in range(0, KC, 8):
                P.dma("sp", dv[:, q:q + 8, csl], xT[sb][:, q:q + 8, :], t_x[sb], reads=[t_x[sb]], final=True)
        kb.pop()
    kb.pop()


def ffn_group_loop(kb, w1, w3, w2, dff, hT, t_h, act, t_act, tm, cnt, res, gate):
    P = kb.P
    W = kb.ws_w
    for f0 in range(0, dff, 1024):
        fw = min(1024, dff - f0)
        nfc = fw // 128
        for c0 in range(f0, f0 + fw, W):
            w1t, w1k = kb.load_w(w1, KC, c0, W)
            w3t, w3k = kb.load_w(w3, KC, c0, W)
            for jj in range(W // 128):
                fc = (c0 - f0) // 128 + jj
                for sb in range(SB2):
                    ba, bb = (fc % 2) * 4 + sb, (fc % 2) * 4 + 2 + sb
                    for kc in range(KC):
                        P.op("pe", MM(kb.bank[ba][:, :], w1t[:, kc, jj * 128:(jj + 1) * 128], hT[sb][:, kc, :], start=(kc == 0), stop=(kc == KC - 1)),
                             reads=[w1k, t_h[sb]], writes=[kb.btok[ba]])
                    for kc in range(KC):
                        P.op("pe", MM(kb.bank[bb][:, :], w3t[:, kc, jj * 128:(jj + 1) * 128], hT[sb][:, kc, :], start=(kc == 0), stop=(kc == KC - 1)),
                             reads=[w3k, t_h[sb]], writes=[kb.btok[bb]])
                    t, tt = tm[cnt["t"] % 4]
                    cnt["t"] += 1
                    P.op("act", ACTF(t[:, :], kb.bank[ba][:, :], AF.Silu), reads=[kb.btok[ba]], writes=[tt])
                    if gate is None:
                        P.op("dve", TT(act[sb][:, fc, :], kb.bank[bb][:, :], t[:, :], ALU.mult), reads=[kb.btok[bb], tt], writes=[t_act[sb]])
                    else:
                        gE, e_, t_gE = gate
                        P.op("dve", TT(t[:, :], kb.bank[bb][:, :], t[:, :], ALU.mult), reads=[kb.btok[bb], tt], writes=[tt])
                        P.op("dve", TT(act[sb][:, fc, :], t[:, :], gE[sb][:, e_, :], ALU.mult), reads=[tt, t_gE[sb]], writes=[t_act[sb]])
        bi = 0
        for g0 in range(0, D, W):
            wt, wtk = kb.load_w(w2[f0:f0 + fw, :], nfc, g0, W)
            for jj in range(W // 128):
                for sb in range(SB2):
                    b = bi % 4
                    bi += 1
                    for k in range(nfc):
                        P.op("pe", MM(kb.bank[b][:, :], wt[:, k, jj * 128:(jj + 1) * 128], act[sb][:, k, :], start=(k == 0), stop=(k == nfc - 1)),
                             reads=[wtk, t_act[sb]], writes=[kb.btok[b]])
                    res[sb]((g0 + jj * 128) // 128, b)


def rope_tables():
    t = np.arange(SEQ)
    rows = SEQ // 64
    pos = np.stack([t // 64 - rows // 2, t % 64 - 32], axis=-1).astype(np.float32)
    inv = (10000.0 ** (-np.arange(32, dtype=np.float32) / 32)).astype(np.float32)
    ang = pos[:, :, None] * inv
    c = np.cos(ang).astype(np.float32)
    s = np.sin(ang).astype(np.float32)
    C = np.zeros((128, SEQ), np.float32)
    S = np.zeros((128, SEQ), np.float32)
    for ax in range(2):
        C[ax * 64:ax * 64 + 32] = c[:, ax].T
        C[ax * 64 + 32:ax * 64 + 64] = c[:, ax].T
        S[ax * 64:ax * 64 + 32] = -s[:, ax].T
        S[ax * 64 + 32:ax * 64 + 64] = s[:, ax].T
    perm = np.zeros((128, 128), np.float32)
    for m in range(128):
        k = m + 32 if (m % 64) < 32 else m - 32
        perm[k, m] = 1.0
    return C, S, perm


def host_layout(inp, b):
    f = lambda a: np.ascontiguousarray(a, dtype=np.float32)
    C, S, perm = rope_tables()
    L = 2
    m = {}
    m["xT"] = f(inp["x"][b].T)
    m["pT"] = f(np.transpose(inp["p"][:, b], (0, 2, 1)))
    m["w_in"] = f(inp["w_in"])
    cols = lambda g: f(np.transpose(g.reshape(L, KC, 128), (0, 2, 1)))
    m["gmix"] = cols(inp["mix_norm"])
    m["gffn"] = cols(inp["ffn_norm"])
    m["gple"] = cols(inp["ple_norm"])
    m["qg"] = f(np.stack([inp["q_norm"], inp["k_norm"]], axis=2))
    m["ropeC"], m["ropeS"], m["perm"] = C, S, perm
    m["ident"] = np.eye(128, dtype=np.float32)
    io = np.arange(512, dtype=np.float32)
    m["iota"] = f(np.broadcast_to(np.stack([io, 511.0 - io])[None], (128, 2, 512)))
    pc = lambda a: f(np.transpose(a.reshape(L, 2, 32, 128), (0, 3, 1, 2)).reshape(L, 128, 64))
    m["lam_re_c"] = pc(inp["ssm_lambda_re"])
    m["lam_im_c"] = pc(inp["ssm_lambda_im"])
    m["logdt_c"] = pc(np.repeat(inp["ssm_log_dt"].reshape(L, 2, 32, 2), 64, axis=3))
    BT = np.zeros((L, 2, 32, 2, 128, 128), np.float32)
    CT = np.zeros((L, 2, 32, 2, 128, 128), np.float32)
    for ri, (bn, cn) in enumerate((("ssm_b_re", "ssm_c_re"), ("ssm_b_im", "ssm_c_im"))):
        Bm, Cm = inp[bn], inp[cn]
        for j in range(32):
            for gi in range(2):
                g = 2 * j + gi
                r0 = 32 * (j % 4) + 16 * gi
                BT[:, :, j, ri, r0:r0 + 16, 64 * gi:64 * gi + 64] = np.transpose(Bm[:, :, g], (0, 1, 3, 2))
                CT[:, :, j, ri, 64 * gi:64 * gi + 64, r0:r0 + 16] = np.transpose(Cm[:, :, g], (0, 1, 3, 2))
    m["BT"] = BT.reshape(L, 64, 2, 128, 128)
    m["CT"] = CT.reshape(L, 64, 2, 128, 128)
    m["dskip"] = f(np.transpose(inp["ssm_d"].reshape(L, 8, 128), (0, 2, 1)))
    m["glu_w"] = f(inp["ssm_glu_w"])
    m["vgain"] = f(np.broadcast_to(inp["gmlp_v_norm"][:, None, :], (L, 128, 1024)))
    m["wsT"] = f(np.transpose(inp["gmlp_ws"], (0, 3, 1, 2)))
    m["bsrow"] = f(inp["gmlp_b"].reshape(L, 1, 1024))
    m["w_branch"] = f(inp["w_branch"].reshape(L, 3072, D))
    m["w_out"] = f(inp["w_out"])
    m["dense_w1"] = f(inp["dense_w1"][0])
    m["dense_w3"] = f(inp["dense_w3"][0])
    m["dense_w2"] = f(inp["dense_w2"][0])
    rwp = np.zeros((128, KC, 128), np.float32)
    rwp[:, :, 0:8] = np.transpose(inp["router_w"][0].reshape(KC, 128, 8), (1, 0, 2))
    m["router_w"] = rwp
    m["expert_w1"] = f(inp["expert_w1"][0])
    m["expert_w3"] = f(inp["expert_w3"][0])
    m["expert_w2"] = f(inp["expert_w2"][0])
    m["ple_gate_w"] = f(inp["ple_gate_w"])
    m["ple_proj_w"] = f(inp["ple_proj_w"])
    return m


_CACHE = {}


def kernel(**inputs):
    depth = 2
    if "nc" not in _CACHE:
        _CACHE["nc"] = build_program(depth)
    nc = _CACHE["nc"]
    inp = {k: np.asarray(v) for k, v in inputs.items()}
    maps = [host_layout(inp, b) for b in range(2)]
    for k in maps[0]:
        if k not in ("xT", "pT"):
            maps[1][k] = maps[0][k]
    res = run_bass_kernel_spmd(nc, maps, core_ids=[0, 1])
    out = np.stack([np.ascontiguousarray(res.results[b]["outT"].T) for b in range(2)], axis=0)
    return out.astype(np.float32)
```

```python
import math
from contextlib import ExitStack

import numpy as np
import ml_dtypes
import concourse.bass as bass
import concourse.mybir as mybir
from concourse.bass_utils import run_bass_kernel_spmd

F32 = mybir.dt.float32
BF16 = mybir.dt.bfloat16
ALU = mybir.AluOpType
AF = mybir.ActivationFunctionType
AX = mybir.AxisListType

D = 2048
KC = 16
NTOK = 1024
SEQ = 4096
N_IN = 10752
EPS = 1e-6
NCORES = 8


class Tok:
    __slots__ = ("lw", "rd", "name", "sem", "cnt")

    def __init__(self, name=""):
        self.lw = None
        self.rd = []
        self.name = name
        self.sem = None
        self.cnt = 0


class Prog:
    ENG = ("pe", "dve", "act", "pool", "sp")

    def __init__(self, nc, stack):
        self.nc = nc
        self.stack = stack
        self.q = {e: [] for e in self.ENG}
        self.sem = {e: stack.enter_context(nc.semaphore("s_" + e)) for e in self.ENG}
        self.cnt = {e: 0 for e in self.ENG}
        self.waited = {e: {} for e in self.ENG}
        self.nsem = 0
        self.final = []
        self.pool = []
        self.free = []
        self.scopes = []

    def _deps(self, eng, reads, writes):
        deps = []
        for b in reads:
            if b.lw is not None:
                deps.append(b.lw)
        for b in writes:
            if b.lw is not None:
                deps.append(b.lw)
            deps.extend(b.rd)
        w = self.waited[eng]
        best = {}
        own = self.sem[eng]
        for (sem, val) in deps:
            if eng == "pe" and sem is own:
                continue
            k = id(sem)
            if w.get(k, 0) < val and best.get(k, (None, 0))[1] < val:
                best[k] = (sem, val)
        waits = []
        for k, (sem, val) in best.items():
            w[k] = val
            waits.append((sem, val))
        return waits

    def _mark(self, tok, reads, writes):
        for b in writes:
            b.lw = tok
            b.rd = []
        for b in reads:
            if b not in writes:
                b.rd.append(tok)
                if len(b.rd) > 48:
                    best = {}
                    for (sem, val) in b.rd:
                        k = id(sem)
                        if best.get(k, (None, 0))[1] < val:
                            best[k] = (sem, val)
                    b.rd = list(best.values())

    def op(self, eng, fn, reads=(), writes=()):
        waits = self._deps(eng, reads, writes)
        self.cnt[eng] += 1
        tok = (self.sem[eng], self.cnt[eng])
        self.q[eng].append((waits, fn, (self.sem[eng], 1)))
        self._mark(tok, reads, writes)
        return tok

    def dma(self, eng, out, in_, owner, reads=(), writes=(), final=False, **kw):
        waits = self._deps(eng, reads, writes)
        if owner.sem is None:
            if self.free:
                idx = self.free.pop()
            else:
                self.nsem += 1
                self.pool.append([self.stack.enter_context(self.nc.semaphore("d%d" % self.nsem)), 0])
                idx = len(self.pool) - 1
            owner.sem = self.pool[idx][0]
            owner.cnt = self.pool[idx][1]
            owner.name = idx
            if self.scopes:
                self.scopes[-1].append(owner)
        owner.cnt += 16
        self.pool[owner.name][1] = owner.cnt
        tok = (owner.sem, owner.cnt)
        self.q[eng].append((waits, (lambda e: e.dma_start(out=out, in_=in_, **kw)), (owner.sem, 16)))
        self._mark(tok, reads, writes)
        if final:
            self.final.append(tok)
        return tok

    def barrier(self):
        toks = [(self.sem[e], self.cnt[e]) for e in self.ENG if self.cnt[e] > 0]
        toks += [(p[0], p[1]) for p in self.pool if p[1] > 0]
        for e in self.ENG:
            waits = []
            w = self.waited[e]
            for (sem, val) in toks:
                if sem is self.sem[e]:
                    continue
                if w.get(id(sem), 0) < val:
                    w[id(sem)] = val
                    waits.append((sem, val))
            if waits:
                self.q[e].append((waits, None, None))

    def push_scope(self):
        self.scopes.append([])

    def pop_scope(self):
        self.barrier()
        for t in self.scopes.pop():
            self.free.append(t.name)
            t.sem = None

    def finish(self):
        best = {}
        for (sem, val) in self.final:
            k = id(sem)
            if best.get(k, (None, 0))[1] < val:
                best[k] = (sem, val)
        self.q["sp"].append((list(best.values()), None, None))

    def emit(self):
        nc = self.nc
        q = self.q

        def replay(name, e):
            for (waits, fn, inc) in q[name]:
                for (sem, val) in waits:
                    e.wait_ge(sem, val)
                if fn is not None:
                    ins = fn(e)
                    if inc is not None:
                        ins.then_inc(inc[0], inc[1])

        with nc.Block() as block:
            @block.tensor
            def _(e):
                replay("pe", e)

            @block.vector
            def _(e):
                replay("dve", e)

            @block.scalar
            def _(e):
                replay("act", e)

            @block.gpsimd
            def _(e):
                replay("pool", e)

            @block.sync
            def _(e):
                replay("sp", e)


def MM(out, lhsT, rhs, start=True, stop=True):
    return lambda e: e.matmul(out, lhsT, rhs, start=start, stop=stop)


def ACTF(out, in_, func, **kw):
    return lambda e: e.activation(out, in_, func, **kw)


def TT(out, a, b, op):
    return lambda e: e.tensor_tensor(out, a, b, op)


def TS(out, a, s1, s2, op0, op1=None):
    if op1 is None:
        return lambda e: e.tensor_scalar(out, a, s1, None, op0)
    return lambda e: e.tensor_scalar(out, a, s1, s2, op0, op1)


def STT(out, in0, scalar, in1, op0, op1):
    return lambda e: e.scalar_tensor_tensor(out, in0, scalar, in1, op0, op1)


def CP(out, in_):
    return lambda e: e.tensor_copy(out, in_)


def MSET(out, v):
    return lambda e: e.memset(out, v)


def RCP(out, in_):
    return lambda e: e.reciprocal(out, in_)


TB = 512
NBLK = SEQ // TB
WS_KC = 16
WS_W = 512


class KB:
    def __init__(self):
        self.nc = bass.Bass("TRN2", target_bir_lowering=False)
        self.st = ExitStack()
        self.P = Prog(self.nc, self.st)
        self.n = 0
        self.bank = [self.st.enter_context(self.nc.psum_tensor("bank%d" % i, [128, 512], F32)) for i in range(8)]
        self.btok = [Tok("bank%d" % i) for i in range(8)]
        self.sstack = [self.st]

    def push(self):
        st = ExitStack()
        self.sstack.append(st)
        self.P.push_scope()

    def pop(self):
        self.P.pop_scope()
        self.sstack.pop().close()

    def sb(self, shape, dt, name=None):
        self.n += 1
        return self.sstack[-1].enter_context(
            self.nc.sbuf_tensor("sb%d_%s" % (self.n, name or "t"), shape, dt))

    def din(self, name, shape, dt=F32):
        return self.nc.dram_tensor(name, list(shape), dt, kind="ExternalInput").ap()

    def dout(self, name, shape, dt=F32):
        return self.nc.dram_tensor(name, list(shape), dt, kind="ExternalOutput").ap()

    def dscr(self, name, shape, dt=F32):
        return self.nc.dram_tensor(name, list(shape), dt).ap()

    def load(self, dram_ap, shape, dt=F32, eng="sp", name=None):
        t = self.sb(shape, dt, name)
        tk = Tok(name or "ld")
        self.P.dma(eng, t[:], dram_ap, tk, writes=[tk])
        return t, tk

    def done(self):
        self.P.finish()
        self.P.emit()
        self.st.close()
        return self.nc

    def init_wslots(self, n=4, width=WS_W):
        self.ws_w = width
        self.wslots = [self.sb([128, WS_KC, width], BF16, "wslot%d" % i) for i in range(n)]
        self.wtok = [Tok("wslot%d" % i) for i in range(n)]
        self.wi = 0

    def load_w(self, w_ap, kc_n, c0, cw):
        i = self.wi % len(self.wslots)
        self.wi += 1
        t, tk = self.wslots[i], self.wtok[i]
        wv = w_ap.rearrange("(kc p) n -> p kc n", p=128)
        for q in range(0, kc_n, 8):
            q1 = min(q + 8, kc_n)
            self.P.dma("pool", t[:, q:q1, 0:cw], wv[:, q:q1, c0:c0 + cw], tk, writes=[tk])
        return t, tk

    def linear(self, w_ap, kc_n, col0, ncols, rhs_fn, rhs_toks, epilogue, banks):
        P = self.P
        bi = 0
        for g0 in range(col0, col0 + ncols, self.ws_w):
            gw = min(self.ws_w, col0 + ncols - g0)
            wt, wtk = self.load_w(w_ap, kc_n, g0, gw)
            for jj in range(gw // 128):
                b = banks[bi % len(banks)]
                bi += 1
                for kc in range(kc_n):
                    P.op("pe", MM(self.bank[b][:, :], wt[:, kc, jj * 128:(jj + 1) * 128], rhs_fn(kc),
                                  start=(kc == 0), stop=(kc == kc_n - 1)),
                         reads=[wtk] + list(rhs_toks), writes=[self.btok[b]])
                epilogue((g0 + jj * 128) // 128, b)

    def linear2(self, w_ap, kc_n, col0, ncols, rhs_fn, rhs_toks, epilogue, banks, nsb):
        P = self.P
        bi = 0
        for g0 in range(col0, col0 + ncols, self.ws_w):
            gw = min(self.ws_w, col0 + ncols - g0)
            wt, wtk = self.load_w(w_ap, kc_n, g0, gw)
            for jj in range(gw // 128):
                for sb in range(nsb):
                    b = banks[bi % len(banks)]
                    bi += 1
                    for kc in range(kc_n):
                        P.op("pe", MM(self.bank[b][:, :], wt[:, kc, jj * 128:(jj + 1) * 128], rhs_fn(kc, sb),
                                      start=(kc == 0), stop=(kc == kc_n - 1)),
                             reads=[wtk, rhs_toks[sb]], writes=[self.btok[b]])
                    epilogue((g0 + jj * 128) // 128, b, sb)

    def consts(self):
        P = self.P
        self.ones32 = self.sb([128, 128], F32, "ones32")
        self.ones_tok = Tok("ones")
        P.op("dve", MSET(self.ones32[:, :], 1.0), writes=[self.ones_tok])
        self.onesb = self.sb([128, 128], BF16, "onesb")
        self.onesb_tok = Tok("onesb")
        P.op("dve", MSET(self.onesb[:, :], 1.0), writes=[self.onesb_tok])
        self.eps_t = self.sb([128, 1], F32, "eps")
        self.eps_tok = Tok("eps")
        P.op("dve", MSET(self.eps_t[:, :], EPS), writes=[self.eps_tok])
        self.sq = [self.sb([128, TB], F32, "sq%d" % i) for i in range(2)]
        self.sqtok = [Tok("sq%d" % i) for i in range(2)]
        self.rstd = self.sb([128, TB], F32, "rstd")
        self.rstok = Tok("rstd")

    def rmsnorm(self, xT, xtok, gcols, gtok, hT, htok, bank, kcn=KC, h32cb=None):
        P = self.P
        sq, sqtok, rstd, rstok = self.sq, self.sqtok, self.rstd, self.rstok
        for kc in range(kcn):
            s = kc % 2
            P.op("act", ACTF(sq[s][:, :], xT[:, kc, :], AF.Square), reads=[xtok], writes=[sqtok[s]])
            P.op("pe", MM(self.bank[bank][:, :], self.ones32[:, :], sq[s][:, :], start=(kc == 0), stop=(kc == kcn - 1)),
                 reads=[sqtok[s], self.ones_tok], writes=[self.btok[bank]])
        P.op("act", ACTF(rstd[:, :], self.bank[bank][:, :], AF.Sqrt, scale=1.0 / (kcn * 128), bias=self.eps_t[:, 0:1]),
             reads=[self.btok[bank], self.eps_tok], writes=[rstok])
        P.op("dve", RCP(rstd[:, :], rstd[:, :]), reads=[rstok], writes=[rstok])
        for kc in range(kcn):
            P.op("dve", STT(hT[:, kc, :], xT[:, kc, :], gcols[:, kc:kc + 1], rstd[:, :], ALU.mult, ALU.mult),
                 reads=[xtok, gtok, rstok], writes=[htok])
            if h32cb is not None:
                h32cb(kc)


def rev_view(ap2d, n):
    pstep = ap2d.ap[0][0]
    return bass.AP(ap2d.tensor, ap2d.offset + (n - 1), [[pstep, 128], [-1, n]])


def build_program(depth=2, dbg=False):
    kb = KB()
    nc, P = kb.nc, kb.P
    L = 2
    xT_d = kb.din("xT", [D, SEQ])
    pT_d = kb.din("pT", [L, 256, SEQ])
    w_in_d = kb.din("w_in", [L, D, N_IN])
    gmix_d = kb.din("gmix", [L, 128, KC])
    gffn_d = kb.din("gffn", [L, 128, KC])
    gple_d = kb.din("gple", [L, 128, KC])
    qg_d = kb.din("qg", [L, 128, 2])
    rc_d = kb.din("ropeC", [128, SEQ])
    rs_d = kb.din("ropeS", [128, SEQ])
    pm_d = kb.din("perm", [128, 128])
    id_d = kb.din("ident", [128, 128])
    iota_d = kb.din("iota", [128, 2, 512])
    lre_d = kb.din("lam_re_c", [L, 128, 64])
    lim_d = kb.din("lam_im_c", [L, 128, 64])
    ldt_d = kb.din("logdt_c", [L, 128, 64])
    bt_d = kb.din("BT", [L, 64, 2, 128, 128])
    ct_d = kb.din("CT", [L, 64, 2, 128, 128])
    dsk_d = kb.din("dskip", [L, 128, 8])
    glu_d = kb.din("glu_w", [L, 1024, 1024])
    vg_d = kb.din("vgain", [L, 128, 1024])
    wst_d = kb.din("wsT", [L, 128, 8, 128])
    bs_d = kb.din("bsrow", [L, 1, 1024])
    wbr_d = kb.din("w_branch", [L, 3072, D])
    wout_d = kb.din("w_out", [L, D, D])
    dw1_d = kb.din("dense_w1", [D, 5632])
    dw3_d = kb.din("dense_w3", [D, 5632])
    dw2_d = kb.din("dense_w2", [5632, D])
    rw_d = kb.din("router_w", [128, KC, 128])
    if depth >= 2:
        ew1_d = kb.din("expert_w1", [8, D, 7168])
        ew3_d = kb.din("expert_w3", [8, D, 7168])
        ew2_d = kb.din("expert_w2", [8, 7168, D])
    else:
        ew1_d = ew3_d = ew2_d = None
    pg_d = kb.din("ple_gate_w", [L, D, D])
    pp_d = kb.din("ple_proj_w", [L, 256, D])
    out_d = kb.dout("outT", [D, SEQ])
    kb.dbg = None
    if dbg:
        kb.dbg = (kb.dout("dbg_x1", [D, TB]), kb.dout("dbg_x2", [D, TB]), kb.dout("dbg_g", [128, 8 * TB]), kb.dout("dbg_lg", [128, 32]))
    qk_s = kb.dscr("qk_s", [1280, SEQ], BF16)
    V_s = kb.dscr("V_s", [SEQ, 256], BF16)
    u_s = kb.dscr("u_s", [1024, SEQ], F32)
    zu_s = kb.dscr("zu_s", [1024, SEQ], F32)
    vn_s = kb.dscr("vn_s", [SEQ, 1024], BF16)
    g_s = kb.dscr("g_s", [6144, SEQ], F32)
    at_s = kb.dscr("at_s", [1024, SEQ], BF16)
    y_s = kb.dscr("y_s", [1024, SEQ], F32)
    x_s = kb.dscr("x_s", [D, SEQ], F32)
    x1_s = kb.dscr("x1_s", [D, SEQ], F32)

    kb.consts()
    pm, pmtok = kb.load(pm_d, [128, 128], name="perm")
    ident, idtok = kb.load(id_d, [128, 128], name="ident")

    for l in range(depth):
        src = xT_d if l == 0 else x_s
        dst = out_d if l == depth - 1 else x_s
        phase_a(kb, l, src, w_in_d[l], gmix_d[l], qg_d[l], rc_d, rs_d, pm, pmtok, vg_d[l],
                qk_s, V_s, u_s, zu_s, vn_s, g_s)
        phase_ssm(kb, l, u_s, y_s, lre_d[l], lim_d[l], ldt_d[l], bt_d[l], ct_d[l], dsk_d[l], iota_d,
                  bg=lambda: attn_gen(kb, qk_s, V_s, at_s))
        moe = (l % 2 == 1)
        phase_c(kb, l, src, x1_s, at_s, y_s, zu_s, vn_s, g_s, glu_d[l], wst_d[l], bs_d[l], wbr_d[l], wout_d[l],
                gffn_d[l], gple_d[l], pg_d[l], pp_d[l], pT_d[l],
                (dw1_d, dw3_d, dw2_d), (rw_d, ew1_d, ew3_d, ew2_d), moe, ident, idtok)
        phase_d(kb, l, x1_s, dst, gffn_d[l], gple_d[l], pg_d[l], pp_d[l], pT_d[l],
                (dw1_d, dw3_d, dw2_d), (rw_d, ew1_d, ew3_d, ew2_d), moe, ident, idtok)
    return kb.done()


def phase_a(kb, l, src, w_in, gmix_d, qg_d, rc_d, rs_d, pm, pmtok, vg_d, qk_s, V_s, u_s, zu_s, vn_s, g_s):
    P = kb.P
    kb.push()
    kb.init_wslots(4, width=256)
    gc, gtok = kb.load(gmix_d, [128, KC], name="gmix")
    qg, qgtok = kb.load(qg_d, [128, 2], name="qg")
    vg, vgtok = kb.load(vg_d, [128, 1024], name="vgain")
    xT = kb.sb([128, KC, TB], F32, "xT")
    xtok = Tok("xT")
    hTs = [kb.sb([128, KC, TB], BF16, "hT%d" % i) for i in range(2)]
    htoks = [Tok("hT%d" % i) for i in range(2)]
    rCs = [kb.sb([128, TB], F32, "ropeC%d" % i) for i in range(2)]
    rSs = [kb.sb([128, TB], F32, "ropeS%d" % i) for i in range(2)]
    rctoks, rstoks = [Tok("rc0"), Tok("rc1")], [Tok("rs0"), Tok("rs1")]
    stg = [kb.sb([128, TB], F32, "stg%d" % i) for i in range(4)]
    stgtok = [Tok("stg%d" % i) for i in range(4)]
    mk = lambda n, dt=F32, w=TB: (kb.sb([128, w], dt, n), Tok(n))
    qf, qftok = mk("qf")
    q2, q2tok = mk("q2")
    qr, qrtok = mk("qr")
    qn, qntok = mk("qn")
    t1, t1tok = mk("t1")
    qo = [mk("qo%d" % i, BF16) for i in range(2)]
    wz = kb.sb([128, KC, 1024], BF16, "wz")
    wztok = Tok("wz")
    wv = kb.sb([128, KC, 256], BF16, "wv")
    wvtok = Tok("wv")
    zg, zgtok = mk("zg", F32, 1024)
    zj, zjtok = mk("zj", F32, 1024)
    ssq = kb.sb([128, 2], F32, "ssq")
    ssqtok = Tok("ssq")
    vno = [mk("vno%d" % i, BF16, 1024) for i in range(2)]
    vo = [mk("vo%d" % i, BF16, 256) for i in range(2)]
    wvv = w_in.rearrange("(kc p) n -> p kc n", p=128)
    for q in range(0, KC, 8):
        P.dma("pool", wv[:, q:q + 8, :], wvv[:, q:q + 8, 1280:1536], wvtok, writes=[wvtok])
    for q in range(0, KC, 4):
        P.dma("pool", wz[:, q:q + 4, :], wvv[:, q:q + 4, 3584:4608], wztok, writes=[wztok])
    cnt = {"s": 0}
    xv = src.rearrange("(kc p) n -> p kc n", p=128)
    for nb2 in range(NBLK // 2):
        c0s = [(nb2 * 2 + sb) * TB for sb in range(2)]
        for sb in range(2):
            c0 = c0s[sb]
            for q in range(0, KC, 8):
                P.dma("sp", xT[:, q:q + 8, :], xv[:, q:q + 8, c0:c0 + TB], xtok, writes=[xtok])
            P.dma("sp", rCs[sb][:, :], rc_d[:, c0:c0 + TB], rctoks[sb], writes=[rctoks[sb]])
            P.dma("sp", rSs[sb][:, :], rs_d[:, c0:c0 + TB], rstoks[sb], writes=[rstoks[sb]])
            kb.rmsnorm(xT, xtok, gc, gtok, hTs[sb], htoks[sb], bank=7)

        def epi_qk(j, b, sb):
            c0 = c0s[sb]
            rC, rS, rctok, rstok_ = rCs[sb], rSs[sb], rctoks[sb], rstoks[sb]
            bk, bt = kb.bank[b], kb.btok[b]
            gi = 0 if j < 8 else 1
            P.op("act", ACTF(qf[:, :], bk[:, :], AF.Identity), reads=[bt], writes=[qftok])
            P.op("act", ACTF(q2[:, :], bk[:, :], AF.Square), reads=[bt], writes=[q2tok])
            P.op("pe", MM(kb.bank[6][:, :], kb.ones32[:, :], q2[:, :]), reads=[q2tok, kb.ones_tok], writes=[kb.btok[6]])
            P.op("act", ACTF(qr[:, :], kb.bank[6][:, :], AF.Sqrt, scale=1.0 / 128, bias=kb.eps_t[:, 0:1]),
                 reads=[kb.btok[6], kb.eps_tok], writes=[qrtok])
            P.op("dve", RCP(qr[:, :], qr[:, :]), reads=[qrtok], writes=[qrtok])
            P.op("dve", STT(qn[:, :], qf[:, :], qg[:, gi:gi + 1], qr[:, :], ALU.mult, ALU.mult),
                 reads=[qftok, qgtok, qrtok], writes=[qntok])
            P.op("pe", MM(kb.bank[6][:, :], pm[:, :], qn[:, :]), reads=[qntok, pmtok], writes=[kb.btok[6]])
            P.op("dve", TT(t1[:, :], qn[:, :], rC[:, :], ALU.mult), reads=[qntok, rctok], writes=[t1tok])
            P.op("dve", TT(qn[:, :], kb.bank[6][:, :], rS[:, :], ALU.mult), reads=[kb.btok[6], rstok_], writes=[qntok])
            o, otok = qo[j % 2]
            P.op("dve", TT(o[:, :], t1[:, :], qn[:, :], ALU.add), reads=[t1tok, qntok], writes=[otok])
            P.dma("sp", qk_s[j * 128:(j + 1) * 128, c0:c0 + TB], o[:, :], otok, reads=[otok])

        def mk_epi_raw(dst_s, j0):
            def epi(j, b, sb):
                c0 = c0s[sb]
                s = cnt["s"] % 4
                cnt["s"] += 1
                if cnt["s"] % 2 == 0:
                    P.op("act", ACTF(stg[s][:, :], kb.bank[b][:, :], AF.Identity), reads=[kb.btok[b]], writes=[stgtok[s]])
                else:
                    P.op("dve", CP(stg[s][:, :], kb.bank[b][:, :]), reads=[kb.btok[b]], writes=[stgtok[s]])
                r0 = (j - j0) * 128
                P.dma("sp", dst_s[r0:r0 + 128, c0:c0 + TB], stg[s][:, :], stgtok[s], reads=[stgtok[s]])
            return epi

        rhs = lambda kc, sb: hTs[sb][:, kc, :]
        kb.linear2(w_in, KC, 0, 1280, rhs, htoks, epi_qk, [0, 1, 2, 3], 2)
        kb.linear2(w_in, KC, 1536, 1024, rhs, htoks, mk_epi_raw(u_s, 12), [0, 1, 2, 3], 2)
        kb.linear2(w_in, KC, 2560, 1024, rhs, htoks, mk_epi_raw(zu_s, 20), [0, 1, 2, 3], 2)
        kb.linear2(w_in, KC, 4608, 6144, rhs, htoks, mk_epi_raw(g_s, 36), [0, 1, 2, 3], 2)
        for tl8 in range(2 * TB // 128):
            sb, tl = tl8 // 4, tl8 % 4
            hT, htok, c0 = hTs[sb], htoks[sb], c0s[sb]
            tsl = slice(tl * 128, (tl + 1) * 128)
            r0 = c0 + tl * 128
            b = 4
            for kc in range(KC):
                P.op("pe", MM(kb.bank[b][:, 0:256], hT[:, kc, tsl], wv[:, kc, :], start=(kc == 0), stop=(kc == KC - 1)),
                     reads=[htok, wvtok], writes=[kb.btok[b]])
            o, otok = vo[tl % 2]
            P.op("dve", CP(o[:, :], kb.bank[b][:, 0:256]), reads=[kb.btok[b]], writes=[otok])
            P.dma("sp", V_s[r0:r0 + 128, :], o[:, :], otok, reads=[otok])
            for half in range(2):
                b = 5 + half
                for kc in range(KC):
                    P.op("pe", MM(kb.bank[b][:, :], hT[:, kc, tsl], wz[:, kc, half * 512:(half + 1) * 512],
                                  start=(kc == 0), stop=(kc == KC - 1)),
                         reads=[htok, wztok], writes=[kb.btok[b]])
                P.op("act", ACTF(zg[:, half * 512:(half + 1) * 512], kb.bank[b][:, :], AF.Gelu_apprx_tanh),
                     reads=[kb.btok[b]], writes=[zgtok])
            P.op("dve", TT(zj[:, :], zg[:, :], zg[:, :], ALU.mult), reads=[zgtok], writes=[zjtok])
            P.op("dve", lambda e: e.reduce_sum(ssq[:, 0:1], zj[:, :], AX.X), reads=[zjtok], writes=[ssqtok])
            P.op("act", ACTF(ssq[:, 1:2], ssq[:, 0:1], AF.Sqrt, scale=1.0 / 1024, bias=kb.eps_t[:, 0:1]),
                 reads=[ssqtok, kb.eps_tok], writes=[ssqtok])
            P.op("dve", RCP(ssq[:, 1:2], ssq[:, 1:2]), reads=[ssqtok], writes=[ssqtok])
            o, otok = vno[tl % 2]
            P.op("dve", STT(o[:, :], zg[:, :], ssq[:, 1:2], vg[:, :], ALU.mult, ALU.mult),
                 reads=[zgtok, ssqtok, vgtok], writes=[otok])
            P.dma("sp", vn_s[r0:r0 + 128, :], o[:, :], otok, reads=[otok])
    kb.pop()


def attn_gen(kb, qk_s, V_s, at_s):
    P = kb.P
    kT = kb.sb([128, 2, SEQ], BF16, "kT")
    ktok = Tok("kT")
    kv = qk_s[1024:1280, :].rearrange("(g p) n -> p g n", p=128)
    for g in range(2):
        P.dma("sp", kT[:, g, :], kv[:, g, :], ktok, writes=[ktok])
    V = kb.sb([128, 32, 256], BF16, "V")
    vtok = Tok("V")
    vv = V_s.rearrange("(kb p) c -> p kb c", p=128)
    for q in range(0, 32, 8):
        P.dma("sp", V[:, q:q + 8, :], vv[:, q:q + 8, :], vtok, writes=[vtok])
    qt = [kb.sb([128, 8, TB], BF16, "q%d" % i) for i in range(2)]
    qtok = [Tok("q%d" % i) for i in range(2)]
    pt = [kb.sb([128, TB], BF16, "pt%d" % i) for i in range(3)]
    pttok = [Tok("pt%d" % i) for i in range(3)]
    rd = kb.sb([128, TB], F32, "rden")
    rdtok = Tok("rden")
    ost = [kb.sb([128, 8, TB], BF16, "ost%d" % i) for i in range(2)]
    ostok = [Tok("ost%d" % i) for i in range(2)]
    scale = 128 ** -0.5
    qv = qk_s[0:1024, :].rearrange("(h p) n -> p h n", p=128)
    av = at_s.rearrange("(h p) n -> p h n", p=128)
    it = 0
    for qb in range(NBLK):
        c0 = qb * TB
        q, qtk = qt[qb % 2], qtok[qb % 2]
        P.dma("sp", q[:, :, :], qv[:, :, c0:c0 + TB], qtk, writes=[qtk])
        os_, ostk = ost[qb % 2], ostok[qb % 2]
        for h in range(8):
            g = h // 4
            ob, db = 5, 6
            def emit_S(kbk_, sbk_):
                P.op("pe", MM(kb.bank[3 + sbk_][:, :], kT[:, g, kbk_ * 128:(kbk_ + 1) * 128], q[:, h, :]),
                     reads=[ktok, qtk], writes=[kb.btok[3 + sbk_]])

            emit_S(0, it % 2)
            for kbk in range(32):
                sbk = it % 2
                it += 1
                sbn = 3 + sbk
                P.op("act", ACTF(pt[sbk][:, :], kb.bank[sbn][:, :], AF.Exp, scale=scale),
                     reads=[kb.btok[sbn]], writes=[pttok[sbk]])
                if kbk + 1 < 32:
                    emit_S(kbk + 1, it % 2)
                P.op("pe", MM(kb.bank[ob][:, :], V[:, kbk, g * 128:(g + 1) * 128], pt[sbk][:, :],
                              start=(kbk == 0), stop=(kbk == 31)),
                     reads=[vtok, pttok[sbk]], writes=[kb.btok[ob]])
                P.op("pe", MM(kb.bank[db][:, :], kb.onesb[:, :], pt[sbk][:, :], start=(kbk == 0), stop=(kbk == 31)),
                     reads=[kb.onesb_tok, pttok[sbk]], writes=[kb.btok[db]])
                yield
            P.op("dve", RCP(rd[:, :], kb.bank[db][:, :]), reads=[kb.btok[db]], writes=[rdtok])
            P.op("dve", TT(os_[:, h, :], kb.bank[ob][:, :], rd[:, :], ALU.mult), reads=[kb.btok[ob], rdtok], writes=[ostk])
        P.dma("sp", av[:, :, c0:c0 + TB], os_[:, :, :], ostk, reads=[ostk])


MAGIC = 12582912.0
TWO_PI = float(2 * np.pi)


def phase_ssm(kb, l, u_s, y_s, lre_d, lim_d, ldt_d, bt_d, ct_d, dsk_d, iota_d, bg=None):
    P = kb.P
    kb.push()
    bgs = [bg() if bg is not None else None]

    def tick(n):
        for _ in range(n):
            if bgs[0] is None:
                return
            try:
                next(bgs[0])
            except StopIteration:
                bgs[0] = None

    NP = 64
    ld = lambda d, n: kb.load(d, [128, NP], name=n)
    lre, t_lre = ld(lre_d, "lre")
    lim, t_lim = ld(lim_d, "lim")
    ldt, t_ldt = ld(ldt_d, "ldt")
    dsk, t_dsk = kb.load(dsk_d, [128, 8], name="dsk")
    iota, t_iota = kb.load(iota_d, [128, 2, 512], name="iota")
    tk = Tok("ssmpre")
    mk = lambda n: kb.sb([128, NP], F32, n)
    dt_, r_, th, kk, sn, cs, a_re, a_im, den, k_re, k_im, tmp, cT, sT, thT = [mk("p%d" % i) for i in range(15)]
    pre_reads = [t_lre, t_lim, t_ldt, tk]

    def op(fn, eng="dve"):
        P.op(eng, fn, reads=pre_reads, writes=[tk])

    def sincos(ang, s_out, c_out):
        op(TS(kk[:, :], ang[:, :], 1.0 / TWO_PI, MAGIC, ALU.mult, ALU.add))
        op(TS(kk[:, :], kk[:, :], MAGIC, None, ALU.subtract))
        op(STT(tmp[:, :], kk[:, :], -TWO_PI, ang[:, :], ALU.mult, ALU.add))
        op(ACTF(s_out[:, :], tmp[:, :], AF.Sin), "act")
        op(TS(tmp[:, :], ang[:, :], float(np.pi / 2), None, ALU.add))
        op(TS(kk[:, :], tmp[:, :], 1.0 / TWO_PI, MAGIC, ALU.mult, ALU.add))
        op(TS(kk[:, :], kk[:, :], MAGIC, None, ALU.subtract))
        op(STT(tmp[:, :], kk[:, :], -TWO_PI, tmp[:, :], ALU.mult, ALU.add))
        op(ACTF(c_out[:, :], tmp[:, :], AF.Sin), "act")

    op(ACTF(dt_[:, :], ldt[:, :], AF.Exp), "act")
    op(TT(r_[:, :], lre[:, :], dt_[:, :], ALU.mult))
    op(ACTF(r_[:, :], r_[:, :], AF.Exp), "act")
    op(TT(th[:, :], lim[:, :], dt_[:, :], ALU.mult))
    sincos(th, sn, cs)
    op(TT(a_re[:, :], r_[:, :], cs[:, :], ALU.mult))
    op(TT(a_im[:, :], r_[:, :], sn[:, :], ALU.mult))
    op(TS(a_re[:, :], a_re[:, :], -1.0, None, ALU.add))
    op(TT(den[:, :], lre[:, :], lre[:, :], ALU.mult))
    op(TT(tmp[:, :], lim[:, :], lim[:, :], ALU.mult))
    op(TT(den[:, :], den[:, :], tmp[:, :], ALU.add))
    op(RCP(den[:, :], den[:, :]))
    op(TT(k_re[:, :], a_re[:, :], lre[:, :], ALU.mult))
    op(TT(tmp[:, :], a_im[:, :], lim[:, :], ALU.mult))
    op(TT(k_re[:, :], k_re[:, :], tmp[:, :], ALU.add))
    op(TT(k_re[:, :], k_re[:, :], den[:, :], ALU.mult))
    op(TT(k_im[:, :], a_im[:, :], lre[:, :], ALU.mult))
    op(TT(tmp[:, :], a_re[:, :], lim[:, :], ALU.mult))
    op(TT(k_im[:, :], k_im[:, :], tmp[:, :], ALU.subtract))
    op(TT(k_im[:, :], k_im[:, :], den[:, :], ALU.mult))
    op(TS(thT[:, :], th[:, :], 512.0, None, ALU.mult))
    sincos(thT, sT, cT)

    mkw = lambda n, dt=F32, w=512: (kb.sb([128, w], dt, n), Tok(n))
    Ec, t_Ec = mkw("Ec")
    Es, t_Es = mkw("Es")
    Dr, t_Dr = mkw("Dr")
    Di, t_Di = mkw("Di")
    ph, t_ph = mkw("ph")
    kq, t_kq = mkw("kq")
    m1, t_m1 = mkw("m1")
    m2, t_m2 = mkw("m2")
    m3, t_m3 = mkw("m3")
    m4, t_m4 = mkw("m4")
    t_ini2, t_ini3 = Tok("ini2"), Tok("ini3")
    zr, t_zr = mkw("zr")
    zi, t_zi = mkw("zi")
    sr = [mkw("sr%d" % i) for i in range(2)]
    si = [mkw("si%d" % i) for i in range(2)]
    Pp = [mkw("P%d" % i, BF16) for i in range(4)]
    ini, t_ini = kb.sb([128, 4], F32, "ini"), Tok("ini")
    BTr, t_BT = kb.sb([128, 2, 128], BF16, "BTs"), Tok("BTs")
    Cf, t_Cf = kb.sb([128, 2, 128], F32, "Cf"), Tok("Cf")
    Cb, t_Cb = kb.sb([128, 3, 128], BF16, "Cb"), Tok("Cb")
    ub, t_ub = kb.sb([128, SEQ], BF16, "ub"), Tok("ub")
    uf, t_uf = kb.sb([128, SEQ], F32, "uf"), Tok("uf")
    ysb, t_y = kb.sb([128, SEQ], F32, "ysb"), Tok("ysb")

    def table(ang_col, iota_ap, s_out, t_s, c_out, t_c):
        P.op("dve", TS(ph[:, :], iota_ap, ang_col, None, ALU.mult), reads=[t_iota, tk], writes=[t_ph])
        P.op("dve", TS(kq[:, :], ph[:, :], 1.0 / TWO_PI, MAGIC, ALU.mult, ALU.add), reads=[t_ph], writes=[t_kq])
        P.op("dve", TS(kq[:, :], kq[:, :], MAGIC, None, ALU.subtract), reads=[t_kq], writes=[t_kq])
        P.op("dve", STT(m1[:, :], kq[:, :], -TWO_PI, ph[:, :], ALU.mult, ALU.add), reads=[t_kq, t_ph], writes=[t_m1])
        P.op("act", ACTF(s_out[:, :], m1[:, :], AF.Sin), reads=[t_m1], writes=[t_s])
        P.op("dve", TS(ph[:, :], ph[:, :], float(np.pi / 2), None, ALU.add), reads=[t_ph], writes=[t_ph])
        P.op("dve", TS(kq[:, :], ph[:, :], 1.0 / TWO_PI, MAGIC, ALU.mult, ALU.add), reads=[t_ph], writes=[t_kq])
        P.op("dve", TS(kq[:, :], kq[:, :], MAGIC, None, ALU.subtract), reads=[t_kq], writes=[t_kq])
        P.op("dve", STT(m1[:, :], kq[:, :], -TWO_PI, ph[:, :], ALU.mult, ALU.add), reads=[t_kq, t_ph], writes=[t_m1])
        P.op("act", ACTF(c_out[:, :], m1[:, :], AF.Sin), reads=[t_m1], writes=[t_c])

    for cb in range(8):
        P.dma("pool", ub[:, :], u_s[cb * 128:(cb + 1) * 128, :], t_ub, writes=[t_ub])
        P.dma("sp", uf[:, :], u_s[cb * 128:(cb + 1) * 128, :], t_uf, writes=[t_uf])
        first = True
        for d in range(2):
            for jb in range(4):
                pd = d * 32 + cb * 4 + jb
                col = slice(pd, pd + 1)
                io = iota[:, d, :]
                table(th[:, col], io, Es, t_Es, Ec, t_Ec)
                P.op("dve", TS(m1[:, :], Es[:, :], k_im[:, col], None, ALU.mult), reads=[t_Es, tk], writes=[t_m1])
                P.op("dve", STT(Dr[:, :], Ec[:, :], k_re[:, col], m1[:, :], ALU.mult, ALU.add), reads=[t_Ec, tk, t_m1], writes=[t_Dr])
                P.op("dve", TS(m1[:, :], Es[:, :], k_re[:, col], None, ALU.mult), reads=[t_Es, tk], writes=[t_m1])
                P.op("dve", STT(Di[:, :], Ec[:, :], k_im[:, col], m1[:, :], ALU.mult, ALU.subtract), reads=[t_Ec, tk, t_m1], writes=[t_Di])
                P.dma("pool", BTr[:, :, :], bt_d[pd].rearrange("r p c -> p r c"), t_BT, writes=[t_BT])
                P.dma("sp", Cf[:, :, :], ct_d[pd].rearrange("r p c -> p r c"), t_Cf, writes=[t_Cf])
                P.op("dve", CP(Cb[:, 0, :], Cf[:, 0, :]), reads=[t_Cf], writes=[t_Cb])
                P.op("dve", TS(Cb[:, 1, :], Cf[:, 0, :], -1.0, None, ALU.mult), reads=[t_Cf], writes=[t_Cb])
                P.op("dve", TS(Cb[:, 2, :], Cf[:, 1, :], -1.0, None, ALU.mult), reads=[t_Cf], writes=[t_Cb])
                chunks = list(range(8)) if d == 0 else list(range(7, -1, -1))
                prev = None
                pend = [None]
                b0, b1 = kb.bank[0], kb.bank[1]

                def emit_B(ch_):
                    c_ = slice(ch_ * 512, (ch_ + 1) * 512)
                    P.op("pe", MM(kb.bank[0][:, :], BTr[:, 0, :], ub[:, c_]), reads=[t_BT, t_ub], writes=[kb.btok[0]])
                    P.op("pe", MM(kb.bank[1][:, :], BTr[:, 1, :], ub[:, c_]), reads=[t_BT, t_ub], writes=[kb.btok[1]])

                emit_B(chunks[0])
                for ci, ch in enumerate(chunks):
                    cs_ = slice(ch * 512, (ch + 1) * 512)
                    P.op("dve", TT(m1[:, :], b0[:, :], Dr[:, :], ALU.mult), reads=[kb.btok[0], t_Dr], writes=[t_m1])
                    P.op("dve", TT(m2[:, :], b1[:, :], Di[:, :], ALU.mult), reads=[kb.btok[1], t_Di], writes=[t_m2])
                    P.op("dve", TT(m3[:, :], b0[:, :], Di[:, :], ALU.mult), reads=[kb.btok[0], t_Di], writes=[t_m3])
                    P.op("dve", TT(m4[:, :], b1[:, :], Dr[:, :], ALU.mult), reads=[kb.btok[1], t_Dr], writes=[t_m4])
                    if ci + 1 < len(chunks):
                        emit_B(chunks[ci + 1])
                    tick(4)
                    P.op("pool", TT(zr[:, :], m1[:, :], m2[:, :], ALU.subtract), reads=[t_m1, t_m2], writes=[t_zr])
                    P.op("pool", TT(zi[:, :], m3[:, :], m4[:, :], ALU.add), reads=[t_m3, t_m4], writes=[t_zi])
                    if pend[0] is not None:
                        pend[0]()
                        pend[0] = None
                    (srt, t_sr), (sit, t_si) = sr[ci % 2], si[ci % 2]
                    if prev is None:
                        init_r, init_i = 0.0, 0.0
                        ireads = []
                    else:
                        (pr_, t_pr), (pi_, t_pi) = prev
                        e_r = pr_[:, 511:512] if d == 0 else pr_[:, 0:1]
                        e_i = pi_[:, 511:512] if d == 0 else pi_[:, 0:1]
                        P.op("dve", TS(ini[:, 2:3], e_i, sT[:, col], None, ALU.mult), reads=[t_pi, tk], writes=[t_ini2])
                        P.op("dve", TS(ini[:, 3:4], e_i, cT[:, col], None, ALU.mult), reads=[t_pi, tk], writes=[t_ini3])
                        P.op("dve", STT(ini[:, 0:1], e_r, cT[:, col], ini[:, 2:3], ALU.mult, ALU.subtract),
                             reads=[t_pr, tk, t_ini2], writes=[t_ini])
                        P.op("dve", STT(ini[:, 1:2], e_r, sT[:, col], ini[:, 3:4], ALU.mult, ALU.add),
                             reads=[t_pr, tk, t_ini3], writes=[t_ini])
                        init_r, init_i = ini[:, 0:1], ini[:, 1:2]
                        ireads = [t_ini]
                    rb = r_[:, col].broadcast_to([128, 512])
                    if d == 0:
                        o_r, o_i, i_r, i_i = srt[:, :], sit[:, :], zr[:, :], zi[:, :]
                    else:
                        o_r, o_i = rev_view(srt[:, :], 512), rev_view(sit[:, :], 512)
                        i_r, i_i = rev_view(zr[:, :], 512), rev_view(zi[:, :], 512)
                    P.op("dve", (lambda o, dd, ii, it_: (lambda e: e.tensor_tensor_scan(o, dd, ii, it_, ALU.mult, ALU.add)))(o_r, rb, i_r, init_r),
                         reads=[tk, t_zr] + ireads, writes=[t_sr])
                    P.op("dve", (lambda o, dd, ii, it_: (lambda e: e.tensor_tensor_scan(o, dd, ii, it_, ALU.mult, ALU.add)))(o_i, rb, i_i, init_i),
                         reads=[tk, t_zi] + ireads, writes=[t_si])
                    prev = ((srt, t_sr), (sit, t_si))
                    for (pi4, a_, ta_, b_, tb_) in ((0, srt, t_sr, Ec, t_Ec), (1, sit, t_si, Es, t_Es),
                                                    (2, sit, t_si, Ec, t_Ec), (3, srt, t_sr, Es, t_Es)):
                        P.op("pool", TT(Pp[pi4][0][:, :], a_[:, :], b_[:, :], ALU.mult), reads=[ta_, tb_], writes=[Pp[pi4][1]])
                    for (pi4, ci3) in ((0, 0), (1, 1), (2, 2), (3, 2)):
                        P.op("pe", MM(kb.bank[2][:, :], Cb[:, ci3, :], Pp[pi4][0][:, :], start=(pi4 == 0), stop=(pi4 == 3)),
                             reads=[t_Cb, Pp[pi4][1]], writes=[kb.btok[2]])
                    def yacc(cs_=cs_, first=first):
                        if first:
                            P.op("dve", STT(ysb[:, cs_], uf[:, cs_], dsk[:, cb:cb + 1], kb.bank[2][:, :], ALU.mult, ALU.add),
                                 reads=[t_uf, t_dsk, kb.btok[2]], writes=[t_y])
                        else:
                            P.op("dve", TT(ysb[:, cs_], kb.bank[2][:, :], ysb[:, cs_], ALU.add), reads=[kb.btok[2], t_y], writes=[t_y])
                    pend[0] = yacc
                if pend[0] is not None:
                    pend[0]()
                    pend[0] = None
                first = False
        P.dma("sp", y_s[cb * 128:(cb + 1) * 128, :], ysb[:, :], t_y, reads=[t_y])
    tick(1 << 30)
    kb.pop()


def phase_c(kb, l, src, x1_s, at_s, y_s, zu_s, vn_s, g_s, glu_d, wst_d, bs_d, wbr_d, wout_d,
            gffn_d, gple_d, pg_d, pp_d, pT_d, dense, moe_w, moe, ident, idtok):
    P = kb.P
    kb.push()
    kb.init_wslots(6, width=256)
    gf, t_gf = kb.load(gffn_d, [128, KC], name="gffn")
    gp, t_gp = kb.load(gple_d, [128, KC], name="gple")
    wsT, t_ws = kb.load(wst_d, [128, 8, 128], BF16, eng="pool", name="wsT")
    bsr, t_bs = kb.load(bs_d, [1, 1024], BF16, eng="pool", name="bsrow")
    xT, t_x = kb.sb([128, KC, TB], F32, "xT"), Tok("xT")
    hT, t_h = kb.sb([128, KC, TB], BF16, "hT"), Tok("hT")
    mkw = lambda n, dt=F32: (kb.sb([128, TB], dt, n), Tok(n))
    tm = [mkw("tm%d" % i) for i in range(3)]
    ma, t_ma = mkw("ma")
    mb, t_mb = mkw("mb")
    cnt = {"t": 0}
    xv = src.rearrange("(kc p) n -> p kc n", p=128)

    def residual_epi(j, b):
        P.op("dve", TT(xT[:, j, :], kb.bank[b][:, :], xT[:, j, :], ALU.add), reads=[kb.btok[b], t_x], writes=[t_x])

    for nb in range(NBLK):
        c0 = nb * TB
        csl = slice(c0, c0 + TB)
        for q in range(0, KC, 8):
            P.dma("sp", xT[:, q:q + 8, :], xv[:, q:q + 8, csl], t_x, writes=[t_x])
        kb.push()
        br, t_br = kb.sb([128, 24, TB], BF16, "br"), [Tok("br%d" % i) for i in range(3)]
        gt = [(kb.sb([128, 3, TB], F32, "gt%d" % i), Tok("gt%d" % i)) for i in range(2)]
        kb.push()
        y32, t_y32 = kb.sb([128, 8, TB], F32, "y32"), Tok("y32")
        yb, t_yb = kb.sb([128, 8, TB], BF16, "yb"), Tok("yb")
        P.dma("sp", y32[:, :, :], y_s.rearrange("(kc p) n -> p kc n", p=128)[:, :, csl], t_y32, writes=[t_y32])
        for kc in range(8):
            P.op("act", ACTF(y32[:, kc, :], y32[:, kc, :], AF.Gelu_apprx_tanh), reads=[t_y32], writes=[t_y32])
            P.op("dve", CP(yb[:, kc, :], y32[:, kc, :]), reads=[t_y32], writes=[t_yb])

        def epi_glu(j, b):
            t, tt = tm[cnt["t"] % 3]
            cnt["t"] += 1
            P.op("act", ACTF(t[:, :], kb.bank[b][:, :], AF.Sigmoid), reads=[kb.btok[b]], writes=[tt])
            P.op("dve", TT(br[:, 8 + j, :], y32[:, j, :], t[:, :], ALU.mult), reads=[t_y32, tt], writes=[t_br[1]])

        kb.linear(glu_d, 8, 0, 1024, lambda kc: yb[:, kc, :], [t_yb], epi_glu, banks=[0, 1, 2, 3])
        kb.pop()
        P.dma("sp", br[:, 0:8, :], at_s.rearrange("(kc p) n -> p kc n", p=128)[:, :, csl], t_br[0], writes=[t_br[0]])
        kb.push()
        zu, t_zu = kb.sb([128, 8, TB], F32, "zu"), Tok("zu")
        vn, t_vn = kb.sb([128, 4, 1024], BF16, "vn"), Tok("vn")
        P.dma("sp", zu[:, :, :], zu_s.rearrange("(kc p) n -> p kc n", p=128)[:, :, csl], t_zu, writes=[t_zu])
        P.dma("sp", vn[:, :, :], vn_s[c0:c0 + TB, :].rearrange("(t p) c -> p t c", p=128), t_vn, writes=[t_vn])
        for kc in range(8):
            P.op("act", ACTF(zu[:, kc, :], zu[:, kc, :], AF.Gelu_apprx_tanh), reads=[t_zu], writes=[t_zu])
        for g in range(8):
            b = g % 4
            for tl in range(4):
                osl = slice(tl * 128, (tl + 1) * 128)
                P.op("pe", MM(kb.bank[b][:, osl], vn[:, tl, g * 128:(g + 1) * 128], wsT[:, g, :], start=True, stop=False),
                     reads=[t_vn, t_ws], writes=[kb.btok[b]])
                P.op("pe", MM(kb.bank[b][:, osl], kb.onesb[0:1, :], bsr[0:1, g * 128:(g + 1) * 128], start=False, stop=True),
                     reads=[kb.onesb_tok, t_bs], writes=[kb.btok[b]])
            P.op("dve", TT(br[:, 16 + g, :], kb.bank[b][:, :], zu[:, g, :], ALU.mult), reads=[kb.btok[b], t_zu], writes=[t_br[2]])
        kb.pop()
        bi = 0
        gview = g_s.rearrange("(n j p) t -> j p n t", n=3, p=128)
        for g0 in range(0, D, kb.ws_w):
            wts = [kb.load_w(wbr_d[n * 1024:(n + 1) * 1024, :], 8, g0, kb.ws_w) for n in range(3)]
            for jj in range(kb.ws_w // 128):
                j = (g0 + jj * 128) // 128
                gtt, t_gt = gt[j % 2]
                P.dma("sp", gtt[:, :, :], gview[j][:, :, csl], t_gt, writes=[t_gt])
                for n in range(3):
                    P.op("act", ACTF(gtt[:, n, :], gtt[:, n, :], AF.Sigmoid), reads=[t_gt], writes=[t_gt])
                banks = [(bi * 3 + n) % 6 for n in range(3)]
                bi += 1
                for n in range(3):
                    for k in range(8):
                        P.op("pe", MM(kb.bank[banks[n]][:, :], wts[n][0][:, k, jj * 128:(jj + 1) * 128], br[:, n * 8 + k, :],
                                      start=(k == 0), stop=(k == 7)),
                             reads=[wts[n][1], t_br[n]], writes=[kb.btok[banks[n]]])
                P.op("dve", TT(ma[:, :], kb.bank[banks[0]][:, :], gtt[:, 0, :], ALU.mult), reads=[kb.btok[banks[0]], t_gt], writes=[t_ma])
                P.op("dve", TT(mb[:, :], kb.bank[banks[1]][:, :], gtt[:, 1, :], ALU.mult), reads=[kb.btok[banks[1]], t_gt], writes=[t_mb])
                P.op("dve", TT(ma[:, :], ma[:, :], mb[:, :], ALU.add), reads=[t_ma, t_mb], writes=[t_ma])
                P.op("dve", TT(mb[:, :], kb.bank[banks[2]][:, :], gtt[:, 2, :], ALU.mult), reads=[kb.btok[banks[2]], t_gt], writes=[t_mb])
                P.op("dve", TT(hT[:, j, :], ma[:, :], mb[:, :], ALU.add), reads=[t_ma, t_mb], writes=[t_h])
        kb.pop()
        kb.linear(wout_d, KC, 0, D, lambda kc: hT[:, kc, :], [t_h], residual_epi, banks=[0, 1, 2, 3])
        if kb.dbg is not None and moe and nb == 0:
            P.dma("sp", kb.dbg[0].rearrange("(kc p) n -> p kc n", p=128), xT[:, :, :], t_x, reads=[t_x], final=True)
        x1v = x1_s.rearrange("(kc p) n -> p kc n", p=128)
        for q in range(0, KC, 8):
            P.dma("sp", x1v[:, q:q + 8, csl], xT[:, q:q + 8, :], t_x, reads=[t_x])
    kb.pop()


SB2 = 2


def phase_d(kb, l, x1_s, dst, gffn_d, gple_d, pg_d, pp_d, pT_d, dense, moe_w, moe, ident, idtok):
    P = kb.P
    kb.push()
    kb.init_wslots(4, width=256)
    gf, t_gf = kb.load(gffn_d, [128, KC], name="gffn")
    gp, t_gp = kb.load(gple_d, [128, KC], name="gple")
    if moe:
        rwh, t_rwh = kb.sb([128, KC, 128], BF16, "rwh"), Tok("rwh")
        rwl, t_rwl = kb.sb([128, KC, 128], BF16, "rwl"), Tok("rwl")
        kb.push()
        rw, t_rw = kb.load(moe_w[0], [128, KC, 128], name="rw")
        P.op("dve", CP(rwh[:, :, :], rw[:, :, :]), reads=[t_rw], writes=[t_rwh])
        P.op("dve", TT(rwl[:, :, :], rw[:, :, :], rwh[:, :, :], ALU.subtract), reads=[t_rw, t_rwh], writes=[t_rwl])
        kb.pop()
    xT = [kb.sb([128, KC, TB], F32, "xT%d" % i) for i in range(SB2)]
    t_x = [Tok("xT%d" % i) for i in range(SB2)]
    hT = [kb.sb([128, KC, TB], BF16, "hT%d" % i) for i in range(SB2)]
    t_h = [Tok("hT%d" % i) for i in range(SB2)]
    mkw = lambda n, dt=F32: (kb.sb([128, TB], dt, n), Tok(n))
    tm = [mkw("tm%d" % i) for i in range(4)]
    cnt = {"t": 0}
    xv = x1_s.rearrange("(kc p) n -> p kc n", p=128)
    dv = dst.rearrange("(kc p) n -> p kc n", p=128)

    def mk_res(sb):
        def residual_epi(j, b):
            P.op("dve", TT(xT[sb][:, j, :], kb.bank[b][:, :], xT[sb][:, j, :], ALU.add), reads=[kb.btok[b], t_x[sb]], writes=[t_x[sb]])
        return residual_epi

    for nb2 in range(NBLK // SB2):
        for sb in range(SB2):
            c0 = (nb2 * SB2 + sb) * TB
            for q in range(0, KC, 8):
                P.dma("sp", xT[sb][:, q:q + 8, :], xv[:, q:q + 8, c0:c0 + TB], t_x[sb], writes=[t_x[sb]])
        kb.push()
        act = [kb.sb([128, 8, TB], BF16, "act%d" % i) for i in range(SB2)]
        t_act = [Tok("act%d" % i) for i in range(SB2)]
        if moe:
            h32 = [mkw("h32_%d" % i) for i in range(2)]
            hlo = [mkw("hlo_%d" % i, BF16) for i in range(2)]
            lg, t_lg = kb.sb([128, 4, 8], F32, "lg"), Tok("lg")
            t8, t_t8 = kb.sb([128, 8], F32, "t8"), Tok("t8")
            sm, t_sm = kb.sb([128, 8], F32, "sm"), Tok("sm")
            ex, t_ex = kb.sb([128, 8], F32, "ex"), Tok("ex")
            mask, t_mask = kb.sb([128, 8], F32, "mask"), Tok("mask")
            wg, t_wg = kb.sb([128, 4, 8], F32, "wg"), Tok("wg")
            wgb, t_wgb = kb.sb([128, 128], F32, "wgb"), Tok("wgb")
            gE = [kb.sb([128, 8, TB], BF16, "gE%d" % i) for i in range(SB2)]
            t_gE = [Tok("gE%d" % i) for i in range(SB2)]
        for sb in range(SB2):
            if not moe:
                kb.rmsnorm(xT[sb], t_x[sb], gf, t_gf, hT[sb], t_h[sb], bank=7)
                continue

            def h32cb(kc, sb=sb):
                hh, t_hh = h32[kc % 2]
                P.op("dve", STT(hh[:, :], xT[sb][:, kc, :], gf[:, kc:kc + 1], kb.rstd[:, :], ALU.mult, ALU.mult),
                     reads=[t_x[sb], t_gf, kb.rstok], writes=[t_hh])
                hl, t_hl = hlo[kc % 2]
                P.op("dve", TT(hl[:, :], hh[:, :], hT[sb][:, kc, :], ALU.subtract), reads=[t_hh, t_h[sb]], writes=[t_hl])
                for tl in range(4):
                    tsl = slice(tl * 128, (tl + 1) * 128)
                    o = kb.bank[tl][:, 0:128]
                    P.op("pe", MM(o, hT[sb][:, kc, tsl], rwh[:, kc, :], start=(kc == 0), stop=False),
                         reads=[t_h[sb], t_rwh], writes=[kb.btok[tl]])
                    P.op("pe", MM(o, hl[:, tsl], rwh[:, kc, :], start=False, stop=False),
                         reads=[t_hl, t_rwh], writes=[kb.btok[tl]])
                    P.op("pe", MM(o, hT[sb][:, kc, tsl], rwl[:, kc, :], start=False, stop=(kc == KC - 1)),
                         reads=[t_h[sb], t_rwl], writes=[kb.btok[tl]])

            kb.rmsnorm(xT[sb], t_x[sb], gf, t_gf, hT[sb], t_h[sb], bank=7, h32cb=h32cb)
            for tl in range(4):
                P.op("dve", CP(lg[:, tl, :], kb.bank[tl][:, 0:8]), reads=[kb.btok[tl]], writes=[t_lg])
            for tl in range(4):
                P.op("dve", lambda e, tl=tl: e.max(t8[:, :], lg[:, tl, :]), reads=[t_lg], writes=[t_t8])
                P.op("dve", TS(sm[:, 0:1], t8[:, 0:1], -1.0, None, ALU.mult), reads=[t_t8], writes=[t_sm])
                P.op("dve", TS(mask[:, :], lg[:, tl, :], t8[:, 1:2], None, ALU.is_ge), reads=[t_lg, t_t8], writes=[t_mask])
                P.op("act", ACTF(ex[:, :], lg[:, tl, :], AF.Exp, bias=sm[:, 0:1]), reads=[t_lg, t_sm], writes=[t_ex])
                P.op("act", ACTF(sm[:, 1:2], t8[:, 1:2], AF.Exp, bias=sm[:, 0:1]), reads=[t_t8, t_sm], writes=[t_sm])
                P.op("dve", TS(sm[:, 2:3], sm[:, 1:2], 1.0, None, ALU.add), reads=[t_sm], writes=[t_sm])
                P.op("dve", RCP(sm[:, 2:3], sm[:, 2:3]), reads=[t_sm], writes=[t_sm])
                P.op("dve", STT(wg[:, tl, :], ex[:, :], sm[:, 2:3], mask[:, :], ALU.mult, ALU.mult),
                     reads=[t_ex, t_sm, t_mask], writes=[t_wg])
            for e_ in range(8):
                b = e_ % 2
                for tl in range(4):
                    P.op("dve", CP(wgb[:, :], wg[:, tl, e_:e_ + 1].broadcast_to([128, 128])), reads=[t_wg], writes=[t_wgb])
                    P.op("pe", MM(kb.bank[b][:, tl * 128:(tl + 1) * 128], wgb[:, :], ident[:, :]), reads=[t_wgb, idtok], writes=[kb.btok[b]])
                P.op("act", ACTF(gE[sb][:, e_, :], kb.bank[b][:, :], AF.Identity), reads=[kb.btok[b]], writes=[t_gE[sb]])
        res = [mk_res(sb) for sb in range(SB2)]
        if not moe:
            w1, w3, w2 = dense
            ffn_group_loop(kb, w1, w3, w2, 5632, hT, t_h, act, t_act, tm, cnt, res, None)
        else:
            rw_, ew1, ew3, ew2 = moe_w
            for e_ in range(8):
                ffn_group_loop(kb, ew1[e_], ew3[e_], ew2[e_], 7168, hT, t_h, act, t_act, tm, cnt, res, (gE, e_, t_gE))
        kb.pop()
        kb.push()
        ppw, t_ppw = kb.sb([128, 2, D], BF16, "ppw"), Tok("ppw")
        pTb, t_pT = kb.sb([128, 2, TB], BF16, "pTb"), Tok("pTb")
        P.dma("pool", ppw[:, :, :], pp_d.rearrange("(kc p) n -> p kc n", p=128), t_ppw, writes=[t_ppw])
        for sb in range(SB2):
            c0 = (nb2 * SB2 + sb) * TB
            csl = slice(c0, c0 + TB)
            kb.rmsnorm(xT[sb], t_x[sb], gp, t_gp, hT[sb], t_h[sb], bank=7)
            P.dma("pool", pTb[:, :, :], pT_d.rearrange("(kc p) n -> p kc n", p=128)[:, :, csl], t_pT, writes=[t_pT])

            def epi_ple(j, b, sb=sb):
                t, tt = tm[cnt["t"] % 4]
                cnt["t"] += 1
                P.op("act", ACTF(t[:, :], kb.bank[b][:, :], AF.Sigmoid), reads=[kb.btok[b]], writes=[tt])
                pb = 4 + (j % 2)
                for k in range(2):
                    P.op("pe", MM(kb.bank[pb][:, :], ppw[:, k, j * 128:(j + 1) * 128], pTb[:, k, :], start=(k == 0), stop=(k == 1)),
                         reads=[t_ppw, t_pT], writes=[kb.btok[pb]])
                P.op("dve", TT(t[:, :], kb.bank[pb][:, :], t[:, :], ALU.mult), reads=[kb.btok[pb], tt], writes=[tt])
                P.op("dve", TT(xT[sb][:, j, :], xT[sb][:, j, :], t[:, :], ALU.add), reads=[t_x[sb], tt], writes=[t_x[sb]])

            kb.linear(pg_d, KC, 0, D, lambda kc, sb=sb: hT[sb][:, kc, :], [t_h[sb]], epi_ple, banks=[0, 1, 2, 3])
            for q in range(0, KC, 8):
                P.dma("sp", dv[:, q:q + 8, csl], xT[sb][:, q:q + 8, :], t_x[sb], reads=[t_x[sb]], final=True)
        kb.pop()
    kb.pop()


def ffn_group_loop(kb, w1, w3, w2, dff, hT, t_h, act, t_act, tm, cnt, res, gate):
    P = kb.P
    W = kb.ws_w
    for f0 in range(0, dff, 1024):
        fw = min(1024, dff - f0)
        nfc = fw // 128
        for c0 in range(f0, f0 + fw, W):
            w1t, w1k = kb.load_w(w1, KC, c0, W)
            w3t, w3k = kb.load_w(w3, KC, c0, W)
            for jj in range(W // 128):
                fc = (c0 - f0) // 128 + jj
                for sb in range(SB2):
                    ba, bb = (fc % 2) * 4 + sb, (fc % 2) * 4 + 2 + sb
                    for kc in range(KC):
                        P.op("pe", MM(kb.bank[ba][:, :], w1t[:, kc, jj * 128:(jj + 1) * 128], hT[sb][:, kc, :], start=(kc == 0), stop=(kc == KC - 1)),
                             reads=[w1k, t_h[sb]], writes=[kb.btok[ba]])
                    for kc in range(KC):
                        P.op("pe", MM(kb.bank[bb][:, :], w3t[:, kc, jj * 128:(jj + 1) * 128], hT[sb][:, kc, :], start=(kc == 0), stop=(kc == KC - 1)),
                             reads=[w3k, t_h[sb]], writes=[kb.btok[bb]])
                    t, tt = tm[cnt["t"] % 4]
                    cnt["t"] += 1
                    P.op("act", ACTF(t[:, :], kb.bank[ba][:, :], AF.Silu), reads=[kb.btok[ba]], writes=[tt])
                    if gate is None:
                        P.op("dve", TT(act[sb][:, fc, :], kb.bank[bb][:, :], t[:, :], ALU.mult), reads=[kb.btok[bb], tt], writes=[t_act[sb]])
                    else:
                        gE, e_, t_gE = gate
                        P.op("dve", TT(t[:, :], kb.bank[bb][:, :], t[:, :], ALU.mult), reads=[kb.btok[bb], tt], writes=[tt])
                        P.op("dve", TT(act[sb][:, fc, :], t[:, :], gE[sb][:, e_, :], ALU.mult), reads=[tt, t_gE[sb]], writes=[t_act[sb]])
        bi = 0
        for g0 in range(0, D, W):
            wt, wtk = kb.load_w(w2[f0:f0 + fw, :], nfc, g0, W)
            for jj in range(W // 128):
                for sb in range(SB2):
                    b = bi % 4
                    bi += 1
                    for k in range(nfc):
                        P.op("pe", MM(kb.bank[b][:, :], wt[:, k, jj * 128:(jj + 1) * 128], act[sb][:, k, :], start=(k == 0), stop=(k == nfc - 1)),
                             reads=[wtk, t_act[sb]], writes=[kb.btok[b]])
                    res[sb]((g0 + jj * 128) // 128, b)


def rope_tables():
    t = np.arange(SEQ)
    rows = SEQ // 64
    pos = np.stack([t // 64 - rows // 2, t % 64 - 32], axis=-1).astype(np.float32)
    inv = (10000.0 ** (-np.arange(32, dtype=np.float32) / 32)).astype(np.float32)
    ang = pos[:, :, None] * inv
    c = np.cos(ang).astype(np.float32)
    s = np.sin(ang).astype(np.float32)
    C = np.zeros((128, SEQ), np.float32)
    S = np.zeros((128, SEQ), np.float32)
    for ax in range(2):
        C[ax * 64:ax * 64 + 32] = c[:, ax].T
        C[ax * 64 + 32:ax * 64 + 64] = c[:, ax].T
        S[ax * 64:ax * 64 + 32] = -s[:, ax].T
        S[ax * 64 + 32:ax * 64 + 64] = s[:, ax].T
    perm = np.zeros((128, 128), np.float32)
    for m in range(128):
        k = m + 32 if (m % 64) < 32 else m - 32
        perm[k, m] = 1.0
    return C, S, perm


def host_layout(inp, b):
    f = lambda a: np.ascontiguousarray(a, dtype=np.float32)
    C, S, perm = rope_tables()
    L = 2
    m = {}
    m["xT"] = f(inp["x"][b].T)
    m["pT"] = f(np.transpose(inp["p"][:, b], (0, 2, 1)))
    m["w_in"] = f(inp["w_in"])
    cols = lambda g: f(np.transpose(g.reshape(L, KC, 128), (0, 2, 1)))
    m["gmix"] = cols(inp["mix_norm"])
    m["gffn"] = cols(inp["ffn_norm"])
    m["gple"] = cols(inp["ple_norm"])
    m["qg"] = f(np.stack([inp["q_norm"], inp["k_norm"]], axis=2))
    m["ropeC"], m["ropeS"], m["perm"] = C, S, perm
    m["ident"] = np.eye(128, dtype=np.float32)
    io = np.arange(512, dtype=np.float32)
    m["iota"] = f(np.broadcast_to(np.stack([io, 511.0 - io])[None], (128, 2, 512)))
    pc = lambda a: f(np.transpose(a.reshape(L, 2, 32, 128), (0, 3, 1, 2)).reshape(L, 128, 64))
    m["lam_re_c"] = pc(inp["ssm_lambda_re"])
    m["lam_im_c"] = pc(inp["ssm_lambda_im"])
    m["logdt_c"] = pc(np.repeat(inp["ssm_log_dt"].reshape(L, 2, 32, 2), 64, axis=3))
    BT = np.zeros((L, 2, 32, 2, 128, 128), np.float32)
    CT = np.zeros((L, 2, 32, 2, 128, 128), np.float32)
    for ri, (bn, cn) in enumerate((("ssm_b_re", "ssm_c_re"), ("ssm_b_im", "ssm_c_im"))):
        Bm, Cm = inp[bn], inp[cn]
        for j in range(32):
            for gi in range(2):
                g = 2 * j + gi
                r0 = 32 * (j % 4) + 16 * gi
                BT[:, :, j, ri, r0:r0 + 16, 64 * gi:64 * gi + 64] = np.transpose(Bm[:, :, g], (0, 1, 3, 2))
                CT[:, :, j, ri, 64 * gi:64 * gi + 64, r0:r0 + 16] = np.transpose(Cm[:, :, g], (0, 1, 3, 2))
    m["BT"] = BT.reshape(L, 64, 2, 128, 128)
    m["CT"] = CT.reshape(L, 64, 2, 128, 128)
    m["dskip"] = f(np.transpose(inp["ssm_d"].reshape(L, 8, 128), (0, 2, 1)))
    m["glu_w"] = f(inp["ssm_glu_w"])
    m["vgain"] = f(np.broadcast_to(inp["gmlp_v_norm"][:, None, :], (L, 128, 1024)))
    m["wsT"] = f(np.transpose(inp["gmlp_ws"], (0, 3, 1, 2)))
    m["bsrow"] = f(inp["gmlp_b"].reshape(L, 1, 1024))
    m["w_branch"] = f(inp["w_branch"].reshape(L, 3072, D))
    m["w_out"] = f(inp["w_out"])
    m["dense_w1"] = f(inp["dense_w1"][0])
    m["dense_w3"] = f(inp["dense_w3"][0])
    m["dense_w2"] = f(inp["dense_w2"][0])
    rwp = np.zeros((128, KC, 128), np.float32)
    rwp[:, :, 0:8] = np.transpose(inp["router_w"][0].reshape(KC, 128, 8), (1, 0, 2))
    m["router_w"] = rwp
    m["expert_w1"] = f(inp["expert_w1"][0])
    m["expert_w3"] = f(inp["expert_w3"][0])
    m["expert_w2"] = f(inp["expert_w2"][0])
    m["ple_gate_w"] = f(inp["ple_gate_w"])
    m["ple_proj_w"] = f(inp["ple_proj_w"])
    return m


_CACHE = {}


def kernel(**inputs):
    depth = 2
    if "nc" not in _CACHE:
        _CACHE["nc"] = build_program(depth)
    nc = _CACHE["nc"]
    inp = {k: np.asarray(v) for k, v in inputs.items()}
    maps = [host_layout(inp, b) for b in range(2)]
    for k in maps[0]:
        if k not in ("xT", "pT"):
            maps[1][k] = maps[0][k]
    res = run_bass_kernel_spmd(nc, maps, core_ids=[0, 1])
    out = np.stack([np.ascontiguousarray(res.results[b]["outT"].T) for b in range(2)], axis=0)
    return out.astype(np.float32)
```
